# Optimizing a Trainium2 kernel written in Bass

```python
import jax, jax.numpy as jnp
from jax import lax
import numpy as np

D_MODEL = 1024
BATCH = 32
SEQ = 2048
DEPTH = 2

N_MEM = 256
EPS = 1e-6
A_WIDTH = D_MODEL // 2
A_CONV = 3
B_HEADS = 8
B_HEAD_DIM = D_MODEL // (2 * B_HEADS)
DILATED_PATTERN = ((128, 1), (512, 4), (2048, 16))
C_HEADS = 8
C_HEAD_DIM = D_MODEL // C_HEADS
C_CONV = 4
C_CHUNK = 64
X_HEADS = 4
X_HEAD_DIM = 64
D_FF = 2816
N_EXPERTS = 8
TOP_K = 2
D_FF_EXPERT = D_FF // 2
N_EVEN = (DEPTH + 1) // 2
N_ODD = DEPTH // 2

kernel_name = 'hybrid_conv_dilated_deltanet_moe_block'


def rms_norm(x, g):
    xf = x.astype(jnp.float32)
    y = xf * lax.rsqrt(jnp.mean(xf * xf, axis=-1, keepdims=True) + EPS)
    return (y * g.astype(jnp.float32)).astype(x.dtype)


def l2_norm(x):
    xf = x.astype(jnp.float32)
    return xf * lax.rsqrt(jnp.sum(xf * xf, axis=-1, keepdims=True) + EPS)


def causal_dwconv(x, w):
    k = w.shape[0]
    return lax.conv_general_dilated(x, w[:, None, :], window_strides=(1,), padding=[(k - 1, 0)],
                                    dimension_numbers=('NWC', 'WIO', 'NWC'),
                                    feature_group_count=x.shape[-1])


def banded_attention(q, k, v, w):
    n, L, h, hd = q.shape
    nb = -(-L // w)
    pad = nb * w - L
    q = jnp.pad(q, ((0, 0), (0, pad), (0, 0), (0, 0))).reshape(n, nb, w, h, hd)
    k = jnp.pad(k, ((0, 0), (w, pad), (0, 0), (0, 0))).reshape(n, nb + 1, w, h, hd)
    v = jnp.pad(v, ((0, 0), (w, pad), (0, 0), (0, 0))).reshape(n, nb + 1, w, h, hd)
    k2 = jnp.concatenate([k[:, :-1], k[:, 1:]], axis=2)
    v2 = jnp.concatenate([v[:, :-1], v[:, 1:]], axis=2)
    s = jnp.einsum('nbqhd,nbkhd->nbhqk', q, k2).astype(jnp.float32) * (hd ** -0.5)
    qi = jnp.arange(w)[:, None]
    kj = jnp.arange(2 * w)[None, :]
    dist = qi + w - kj
    kpos = jnp.arange(nb)[:, None, None] * w + kj[None] - w
    valid = (dist >= 0) & (dist <= w) & (kpos >= 0)
    s = jnp.where(valid[None, :, None], s, -jnp.inf)
    m = jnp.max(s, axis=-1, keepdims=True)
    p = jnp.exp(s - m)
    den = jnp.sum(p, axis=-1)
    o = jnp.einsum('nbhqk,nbkhd->nbqhd', p, v2.astype(jnp.float32))
    o = o / jnp.moveaxis(den, 2, 3)[..., None]
    lse = m[..., 0] + jnp.log(den)
    o = o.reshape(n, nb * w, h, hd)[:, :L]
    lse = jnp.moveaxis(lse, 2, 3).reshape(n, nb * w, h)[:, :L]
    return o, lse


def dilated_attention(q, k, v):
    b, s, h, hd = q.shape
    outs, lses = [], []
    for window, d in DILATED_PATTERN:
        L = s // d
        def to_sub(t):
            return t.reshape(b, L, d, h, hd).transpose(0, 2, 1, 3, 4).reshape(b * d, L, h, hd)
        o, lse = banded_attention(to_sub(q), to_sub(k), to_sub(v), window // d)
        outs.append(o.reshape(b, d, L, h, hd).transpose(0, 2, 1, 3, 4).reshape(b, s, h, hd))
        lses.append(lse.reshape(b, d, L, h).transpose(0, 2, 1, 3).reshape(b, s, h))
    wts = jax.nn.softmax(jnp.stack(lses), axis=0)
    return jnp.einsum('gbsh,gbshd->bshd', wts, jnp.stack(outs))


def conv_dilated_mixer(u, w_in, conv_w, q_g, k_g, w_out):
    b, s, _ = u.shape
    gb, gc, xc, q, k, v = jnp.split(u @ w_in, 6, axis=-1)
    a = gb * causal_dwconv(gc * xc, conv_w)
    q = rms_norm(q.reshape(b, s, B_HEADS, B_HEAD_DIM), q_g)
    k = rms_norm(k.reshape(b, s, B_HEADS, B_HEAD_DIM), k_g)
    v = v.reshape(b, s, B_HEADS, B_HEAD_DIM)
    o = dilated_attention(q, k, v).astype(u.dtype).reshape(b, s, B_HEADS * B_HEAD_DIM)
    return jnp.concatenate([a, o], axis=-1) @ w_out


def gated_delta_rule(q, k, v, beta, g):
    b, s, h, dk = q.shape
    dv = v.shape[-1]
    nc = s // C_CHUNK
    def chunk(t):
        t = t.astype(jnp.float32).reshape((b, nc, C_CHUNK) + t.shape[2:])
        return jnp.moveaxis(t, 3, 2)
    q = chunk(l2_norm(q)) * (dk ** -0.5)
    k = chunk(l2_norm(k))
    v = chunk(v)
    beta = chunk(beta)
    gc = jnp.cumsum(chunk(g), axis=-1)
    causal = jnp.tril(jnp.ones((C_CHUNK, C_CHUNK), dtype=bool))
    strict = jnp.tril(jnp.ones((C_CHUNK, C_CHUNK), dtype=bool), -1)
    decay = jnp.exp(jnp.where(causal, gc[..., :, None] - gc[..., None, :], -jnp.inf))
    kk = jnp.einsum('bnhid,bnhjd->bnhij', k, k)
    a_mat = jnp.where(strict, beta[..., :, None] * kk * decay, 0.0) + jnp.eye(C_CHUNK, dtype=jnp.float32)
    u = lax.linalg.triangular_solve(a_mat, v * beta[..., None], left_side=True, lower=True, unit_diagonal=True)
    w = lax.linalg.triangular_solve(a_mat, k * (beta * jnp.exp(gc))[..., None], left_side=True, lower=True,
                                    unit_diagonal=True)
    attn = jnp.einsum('bnhid,bnhjd->bnhij', q, k) * decay
    q_dec = q * jnp.exp(gc)[..., None]
    k_dec = k * jnp.exp(gc[..., -1:] - gc)[..., None]
    g_last = jnp.exp(gc[..., -1])

    def step(state, inp):
        qd, kd, uc, wc, at, gl = inp
        v_new = uc - jnp.einsum('bhcd,bhde->bhce', wc, state)
        o = jnp.einsum('bhcd,bhde->bhce', qd, state) + jnp.einsum('bhij,bhje->bhie', at, v_new)
        state = state * gl[..., None, None] + jnp.einsum('bhcd,bhce->bhde', kd, v_new)
        return state, o

    xs = (jnp.moveaxis(q_dec, 1, 0), jnp.moveaxis(k_dec, 1, 0), jnp.moveaxis(u, 1, 0),
          jnp.moveaxis(w, 1, 0), jnp.moveaxis(attn, 1, 0), jnp.moveaxis(g_last, 1, 0))
    state0 = jnp.zeros((b, h, dk, dv), jnp.float32)
    _, o = lax.scan(step, state0, xs)
    return jnp.moveaxis(jnp.moveaxis(o, 0, 1), 2, 3).reshape(b, s, h, dv)


def deltanet_mixer(u, w_in, conv_w, a_log, dt_bias, o_g, w_out):
    b, s, _ = u.shape
    proj = u @ w_in
    qkv, gate, bb, aa = jnp.split(proj, [3 * D_MODEL, 4 * D_MODEL, 4 * D_MODEL + C_HEADS], axis=-1)
    qkv = jax.nn.silu(causal_dwconv(qkv, conv_w))
    q, k, v = [t.reshape(b, s, C_HEADS, C_HEAD_DIM) for t in jnp.split(qkv, 3, axis=-1)]
    beta = jax.nn.sigmoid(bb.astype(jnp.float32))
    g = -jnp.exp(a_log.astype(jnp.float32)) * jax.nn.softplus(aa.astype(jnp.float32) + dt_bias.astype(jnp.float32))
    o = gated_delta_rule(q, k, v, beta, g)
    o = rms_norm(o, o_g).astype(u.dtype) * jax.nn.silu(gate.reshape(b, s, C_HEADS, C_HEAD_DIM))
    return o.reshape(b, s, C_HEADS * C_HEAD_DIM) @ w_out


def memory_cross_attention(u, mem_n, w_q, w_kv, q_g, k_g, w_o):
    b, s, _ = u.shape
    m = mem_n.shape[1]
    q = rms_norm((u @ w_q).reshape(b, s, X_HEADS, X_HEAD_DIM), q_g)
    k, v = jnp.split(mem_n @ w_kv, 2, axis=-1)
    k = rms_norm(k.reshape(b, m, X_HEADS, X_HEAD_DIM), k_g)
    v = v.reshape(b, m, X_HEADS, X_HEAD_DIM)
    sc = jnp.einsum('bshd,bmhd->bhsm', q, k).astype(jnp.float32) * (X_HEAD_DIM ** -0.5)
    p = jax.nn.softmax(sc, axis=-1).astype(v.dtype)
    o = jnp.einsum('bhsm,bmhd->bshd', p, v).reshape(b, s, X_HEADS * X_HEAD_DIM)
    return o @ w_o


def swiglu(u, w_gu, w_down):
    gt, up = jnp.split(u @ w_gu, 2, axis=-1)
    return (jax.nn.silu(gt) * up) @ w_down


def moe_swiglu(u, router, w_gu, w_down):
    logits = (u @ router).astype(jnp.float32)
    top_val, top_idx = lax.top_k(logits, TOP_K)
    top_w = jax.nn.softmax(top_val, axis=-1)
    combine = jnp.sum(jax.nn.one_hot(top_idx, N_EXPERTS, dtype=jnp.float32) * top_w[..., None], axis=-2)
    y = jnp.zeros_like(u)
    for e in range(N_EXPERTS):
        y = y + combine[..., e:e + 1].astype(u.dtype) * swiglu(u, w_gu[e], w_down[e])
    return y


def setup_inputs(seed: int = 0) -> dict:
    key = jax.random.key(seed)
    ks = iter(jax.random.split(key, 40))
    def nrm(shape, scale):
        return jax.random.normal(next(ks), shape, jnp.float32) * scale
    def gain(shape):
        return 1.0 + 0.02 * jax.random.normal(next(ks), shape, jnp.float32)
    d = D_MODEL
    dt = jnp.exp(jax.random.uniform(next(ks), (N_ODD, C_HEADS), jnp.float32, np.log(1e-3), np.log(1e-1)))
    return {
        'x': nrm((BATCH, SEQ, d), 1.0),
        'mem': nrm((BATCH, N_MEM, d), 1.0),
        'norm_mix': gain((DEPTH, d)),
        'norm_xattn': gain((DEPTH, d)),
        'norm_mem': gain((DEPTH, d)),
        'norm_ffn': gain((DEPTH, d)),
        'ev_w_in': nrm((N_EVEN, d, 3 * A_WIDTH + 3 * B_HEADS * B_HEAD_DIM), d ** -0.5),
        'ev_conv': nrm((N_EVEN, A_CONV, A_WIDTH), A_CONV ** -0.5),
        'ev_q_norm': gain((N_EVEN, B_HEAD_DIM)),
        'ev_k_norm': gain((N_EVEN, B_HEAD_DIM)),
        'ev_w_out': nrm((N_EVEN, A_WIDTH + B_HEADS * B_HEAD_DIM, d), (A_WIDTH + B_HEADS * B_HEAD_DIM) ** -0.5),
        'od_w_in': nrm((N_ODD, d, 4 * C_HEADS * C_HEAD_DIM + 2 * C_HEADS), d ** -0.5),
        'od_conv': nrm((N_ODD, C_CONV, 3 * C_HEADS * C_HEAD_DIM), C_CONV ** -0.5),
        'od_a_log': jnp.log(jax.random.uniform(next(ks), (N_ODD, C_HEADS), jnp.float32, 1.0, 16.0)),
        'od_dt_bias': jnp.log(jnp.expm1(dt)),
        'od_o_norm': gain((N_ODD, C_HEAD_DIM)),
        'od_w_out': nrm((N_ODD, C_HEADS * C_HEAD_DIM, d), (C_HEADS * C_HEAD_DIM) ** -0.5),
        'xa_w_q': nrm((DEPTH, d, X_HEADS * X_HEAD_DIM), d ** -0.5),
        'xa_w_kv': nrm((DEPTH, d, 2 * X_HEADS * X_HEAD_DIM), d ** -0.5),
        'xa_q_norm': gain((DEPTH, X_HEAD_DIM)),
        'xa_k_norm': gain((DEPTH, X_HEAD_DIM)),
        'xa_w_o': nrm((DEPTH, X_HEADS * X_HEAD_DIM, d), (X_HEADS * X_HEAD_DIM) ** -0.5),
        'ff_w_gu': nrm((N_EVEN, d, 2 * D_FF), d ** -0.5),
        'ff_w_down': nrm((N_EVEN, D_FF, d), D_FF ** -0.5),
        'moe_router': nrm((N_ODD, d, N_EXPERTS), d ** -0.5),
        'moe_w_gu': nrm((N_ODD, N_EXPERTS, d, 2 * D_FF_EXPERT), d ** -0.5),
        'moe_w_down': nrm((N_ODD, N_EXPERTS, D_FF_EXPERT, d), D_FF_EXPERT ** -0.5),
    }


def reference(x, mem, norm_mix, norm_xattn, norm_mem, norm_ffn,
              ev_w_in, ev_conv, ev_q_norm, ev_k_norm, ev_w_out,
              od_w_in, od_conv, od_a_log, od_dt_bias, od_o_norm, od_w_out,
              xa_w_q, xa_w_kv, xa_q_norm, xa_k_norm, xa_w_o,
              ff_w_gu, ff_w_down, moe_router, moe_w_gu, moe_w_down):
    h = x
    for layer in range(DEPTH):
        i = layer // 2
        u = rms_norm(h, norm_mix[layer])
        if layer % 2 == 0:
            h = h + conv_dilated_mixer(u, ev_w_in[i], ev_conv[i], ev_q_norm[i], ev_k_norm[i], ev_w_out[i])
        else:
            h = h + deltanet_mixer(u, od_w_in[i], od_conv[i], od_a_log[i], od_dt_bias[i], od_o_norm[i], od_w_out[i])
        h = h + memory_cross_attention(rms_norm(h, norm_xattn[layer]), rms_norm(mem, norm_mem[layer]),
                                       xa_w_q[layer], xa_w_kv[layer], xa_q_norm[layer], xa_k_norm[layer],
                                       xa_w_o[layer])
        u = rms_norm(h, norm_ffn[layer])
        if layer % 2 == 0:
            h = h + swiglu(u, ff_w_gu[i], ff_w_down[i])
        else:
            h = h + moe_swiglu(u, moe_router[i], moe_w_gu[i], moe_w_down[i])
    return h
```

```python
import contextlib
import numpy as np
import concourse.bass as bass
import concourse.mybir as mybir
from concourse.bass_utils import run_bass_kernel_spmd

F32 = mybir.dt.float32
BF16 = mybir.dt.bfloat16
AF = mybir.ActivationFunctionType
ALU = mybir.AluOpType
AX = mybir.AxisListType


class Buf:
    __slots__ = ("name", "w", "r", "excl")

    def __init__(self, name="", excl=False):
        self.name = name
        self.excl = excl
        self.w = []
        self.r = []


class V:
    __slots__ = ("ap", "bufs")

    def __init__(self, ap, bufs):
        self.ap = ap
        self.bufs = tuple(bufs)

    def __getitem__(self, idx):
        return V(self.ap[idx], self.bufs)

    def bitcast(self, dt):
        return V(self.ap.bitcast(dt), self.bufs)

    def rearrange(self, s, **kw):
        return V(self.ap.rearrange(s, **kw), self.bufs)

    def bc(self, shape):
        return V(self.ap.to_broadcast(shape), self.bufs)

    def on(self, *bufs):
        return V(self.ap, bufs)

    @property
    def shape(self):
        return self.ap.shape


class Eng:
    def __init__(self, name, h, sem, self_sync):
        self.name, self.h, self.sem, self.self_sync = name, h, sem, self_sync
        self.cnt = 0
        self.known = {}


class K:
    def __init__(self, nc, es, n_dma_sems=40):
        self.nc, self.es = nc, es
        mk = lambda n: es.enter_context(nc.semaphore(n))
        self.pe = Eng("pe", nc.tensor, mk("s_pe"), False)
        self.act = Eng("act", nc.scalar, mk("s_act"), True)
        self.dve = Eng("dve", nc.vector, mk("s_dve"), True)
        self.pool = Eng("pool", nc.gpsimd, mk("s_pool"), True)
        self.sp = Eng("sp", nc.sync, mk("s_sp"), False)
        self.engs = [self.pe, self.act, self.dve, self.pool, self.sp]
        self.dsems = [[mk(f"s_d{i}"), 0] for i in range(n_dma_sems)]
        self.dnext = 0
        self.ps = []
        for i in range(8):
            t = es.enter_context(nc.psum_tensor(f"psb{i}", [128, 512], F32))
            self.ps.append(V(t[:, :], [Buf(f"ps{i}", excl=True)]))
        self.psn = 0
        self.held = set()
        self.ntile = 0
        self.ninst = 0

    @contextlib.contextmanager
    def scope(self):
        with contextlib.ExitStack() as es:
            yield es
            self.barrier()

    def psum(self, hold=False):
        while (self.psn % 8) in self.held:
            self.psn += 1
        i = self.psn % 8
        self.psn += 1
        if hold:
            self.held.add(i)
        return self.ps[i]

    def release(self, p):
        self.held.discard(self.ps.index(p))

    def sb(self, shape, dt, name=None, es=None):
        self.ntile += 1
        name = name or f"t{self.ntile}"
        t = (es or self.es).enter_context(self.nc.sbuf_tensor(f"{name}_{self.ntile}", list(shape), dt))
        idx = tuple(slice(None) for _ in shape)
        return V(t[idx], [Buf(name)])

    def dram(self, name, shape, dt, kind="Internal"):
        t = self.nc.dram_tensor(name, list(shape), dt, kind=kind)
        return V(t.ap(), [Buf(name)])

    def _deps(self, e, reads, writes):
        deps = {}

        def need(tok):
            s, v = tok
            k = id(s)
            if k not in deps or deps[k][1] < v:
                deps[k] = (s, v)
        for b in reads:
            for tok in b.w:
                need(tok)
            if b.excl:
                for tok in b.r:
                    need(tok)
        for b in writes:
            for tok in b.w:
                need(tok)
            for tok in b.r:
                need(tok)
        for k, (s, v) in deps.items():
            if s is e.sem and not e.self_sync:
                continue
            if e.known.get(k, 0) >= v:
                continue
            e.h.wait_ge(s, v)
            e.known[k] = v

    def _mark(self, tok, reads, writes):
        for b in writes:
            b.w = [tok]
            b.r = []
        ws = set(id(b) for b in writes)
        for b in reads:
            if id(b) in ws:
                continue
            b.r = [x for x in b.r if x[0] is not tok[0]] + [tok]

    def op(self, e, emit, ins, outs, rd=(), wr=()):
        reads, writes = list(rd), list(wr)
        for v in ins:
            if isinstance(v, V):
                reads.extend(v.bufs)
        for v in outs:
            writes.extend(v.bufs)
        self._deps(e, reads, writes)
        ins_ = emit()
        e.cnt += 1
        ins_.then_inc(e.sem, 1)
        self._mark((e.sem, e.cnt), reads, writes)
        self.ninst += 1
        return ins_

    def dma(self, out, in_, q=None, rd=(), wr=()):
        e = q or self.sp
        reads = list(in_.bufs) + list(rd)
        writes = list(out.bufs) + list(wr)
        ds = self.dsems[self.dnext % len(self.dsems)]
        self.dnext += 1
        self._deps(e, reads, writes)
        if ds[1] > 0 and e.known.get(id(ds[0]), 0) < ds[1]:
            e.h.wait_ge(ds[0], ds[1])
            e.known[id(ds[0])] = ds[1]
        ins_ = e.h.dma_start(out=out.ap, in_=in_.ap)
        ds[1] += 16
        ins_.then_inc(ds[0], 16)
        self._mark((ds[0], ds[1]), reads, writes)
        self.ninst += 1

    def barrier(self):
        toks = [(x.sem, x.cnt) for x in self.engs if x.cnt > 0] + [(d[0], d[1]) for d in self.dsems if d[1] > 0]
        for e in self.engs:
            for s, v in toks:
                if s is e.sem:
                    continue
                if e.known.get(id(s), 0) >= v:
                    continue
                e.h.wait_ge(s, v)
                e.known[id(s)] = v

    @staticmethod
    def _a(x):
        return x.ap if isinstance(x, V) else x

    def mm(self, out, lhsT, rhs, start=True, stop=True):
        return self.op(self.pe, lambda: self.nc.tensor.matmul(out.ap, lhsT.ap, rhs.ap, start=start, stop=stop),
                       [lhsT, rhs], [out])

    def tr(self, out, in_, ident):
        return self.op(self.pe, lambda: self.nc.tensor.transpose(out.ap, in_.ap, ident.ap), [in_, ident], [out])

    def activation(self, out, in_, func, bias=None, scale=None, accum_out=None, e=None):
        e = e or self.act
        kw = {}
        if bias is not None:
            kw["bias"] = self._a(bias)
        if scale is not None:
            kw["scale"] = self._a(scale)
        outs = [out]
        if accum_out is not None:
            kw["accum_out"] = accum_out.ap
            outs.append(accum_out)
        return self.op(e, lambda: e.h.activation(out=out.ap, in_=in_.ap, func=func, **kw),
                       [in_, bias, scale], outs)

    def tt(self, out, in0, in1, op, e=None):
        e = e or self.dve
        return self.op(e, lambda: e.h.tensor_tensor(out=out.ap, in0=in0.ap, in1=in1.ap, op=op), [in0, in1], [out])

    def ts(self, out, in0, s1, op0, s2=None, op1=None, e=None, accum_out=None):
        e = e or self.dve
        kw = {}
        if op1 is not None:
            kw["op1"] = op1
        outs = [out]
        if accum_out is not None:
            kw["accum_out"] = accum_out.ap
            outs.append(accum_out)
        return self.op(e, lambda: e.h.tensor_scalar(out=out.ap, in0=in0.ap, scalar1=self._a(s1), scalar2=self._a(s2),
                                                    op0=op0, **kw), [in0, s1, s2], outs)

    def stt(self, out, in0, scalar, in1, op0, op1, e=None):
        e = e or self.dve
        return self.op(e, lambda: e.h.scalar_tensor_tensor(out=out.ap, in0=in0.ap, scalar=self._a(scalar), in1=in1.ap,
                                                           op0=op0, op1=op1), [in0, scalar, in1], [out])

    def copy(self, out, in_, e=None):
        e = e or self.dve
        if e is self.act:
            return self.op(e, lambda: e.h.copy(out=out.ap, in_=in_.ap), [in_], [out])
        return self.op(e, lambda: e.h.tensor_copy(out=out.ap, in_=in_.ap), [in_], [out])

    def memset(self, out, val, e=None):
        e = e or self.dve
        return self.op(e, lambda: e.h.memset(out.ap, val), [], [out])

    def recip(self, out, in_, e=None):
        e = e or self.dve
        return self.op(e, lambda: e.h.reciprocal(out=out.ap, in_=in_.ap), [in_], [out])

    def recip_fast(self, out, in_):
        e = self.dve
        return self.op(e, lambda: e.h.reciprocal_approx_fast(out=out.ap, in_=in_.ap), [in_], [out])

    def reduce(self, out, in_, op, axis=AX.X, e=None):
        e = e or self.dve
        return self.op(e, lambda: e.h.tensor_reduce(out=out.ap, in_=in_.ap, axis=axis, op=op), [in_], [out])

    def max8(self, out, in_):
        e = self.dve
        return self.op(e, lambda: e.h.max(out=out.ap, in_=in_.ap), [in_], [out])


S = 2048
D = 1024
NMEM = 256
EPS = 1e-6
DFF = 2816
DFE = 1408
NEXP = 8

CST = {}
_off = 0
def _reg(name, w):
    global _off
    CST[name] = (_off, w)
    _off += w
for _n in ("norm_mix", "norm_xattn", "norm_mem", "norm_ffn"):
    for _l in range(2):
        _reg(f"{_n}{_l}", 8)
_reg("ev_conv", 12); _reg("ev_qn", 1); _reg("ev_kn", 1)
for _l in range(2):
    _reg(f"xa_qn{_l}", 1); _reg(f"xa_kn{_l}", 1)
_reg("od_conv", 96); _reg("od_alog", 8); _reg("od_dtb", 8); _reg("od_on", 1)
_reg("ident", 128); _reg("bones64", 128); _reg("ones", 128); _reg("mask", 256)
_reg("tri_incl", 128)
_reg("blk64", 128)
_reg("strict_ji", 128)
_reg("causal_ji", 128)
_reg("strict_ij", 128)
_reg("gtk", 128)
NCST = _off


def make_consts(inp):
    c = np.zeros((128, NCST), np.float32)
    def put(name, arr):
        o, w = CST[name]
        assert arr.shape == (128, w), (name, arr.shape, w)
        c[:, o:o + w] = arr
    for n in ("norm_mix", "norm_xattn", "norm_mem", "norm_ffn"):
        for l in range(2):
            put(f"{n}{l}", np.asarray(inp[n][l]).reshape(8, 128).T)
    put("ev_conv", np.asarray(inp["ev_conv"][0]).reshape(3, 4, 128).transpose(2, 1, 0).reshape(128, 12))
    put("ev_qn", np.tile(np.asarray(inp["ev_q_norm"][0]), 2).reshape(128, 1))
    put("ev_kn", np.tile(np.asarray(inp["ev_k_norm"][0]), 2).reshape(128, 1))
    for l in range(2):
        put(f"xa_qn{l}", np.tile(np.asarray(inp["xa_q_norm"][l]), 2).reshape(128, 1))
        put(f"xa_kn{l}", np.tile(np.asarray(inp["xa_k_norm"][l]), 2).reshape(128, 1))
    put("od_conv", np.asarray(inp["od_conv"][0]).reshape(4, 24, 128).transpose(2, 1, 0).reshape(128, 96))
    put("od_alog", np.broadcast_to(np.asarray(inp["od_a_log"][0]).reshape(1, 8), (128, 8)))
    put("od_dtb", np.broadcast_to(np.asarray(inp["od_dt_bias"][0]).reshape(1, 8), (128, 8)))
    put("od_on", np.asarray(inp["od_o_norm"][0]).reshape(128, 1))
    i = np.arange(128)
    put("ident", np.eye(128, dtype=np.float32))
    put("bones64", (i[:, None] // 64 == i[None, :] // 64).astype(np.float32))
    put("ones", np.ones((128, 128), np.float32))
    msame = (i[:, None] <= i[None, :]).astype(np.float32)
    mprev = (i[:, None] >= i[None, :]).astype(np.float32)
    put("mask", np.concatenate([msame, mprev], 1))
    same = (i[:, None] // 64 == i[None, :] // 64)
    put("tri_incl", ((i[:, None] <= i[None, :]) & same).astype(np.float32))
    put("blk64", same.astype(np.float32))
    put("strict_ji", ((i[:, None] < i[None, :]) & same).astype(np.float32))
    put("causal_ji", ((i[:, None] <= i[None, :]) & same).astype(np.float32))
    put("strict_ij", ((i[None, :] < i[:, None]) & same).astype(np.float32))
    put("gtk", (i[:, None] > i[None, :]).astype(np.float32))
    return c


WEIGHTS = [
    ("ev_w_in", 1024, 3072, "norm_mix0"), ("ev_w_out", 1024, 1024, None),
    ("od_w_in", 1024, 4112, "norm_mix1"), ("od_w_out", 1024, 1024, None),
    ("xa_w_q0", 1024, 256, "norm_xattn0"), ("xa_w_kv0", 1024, 512, "norm_mem0"), ("xa_w_o0", 256, 1024, None),
    ("xa_w_q1", 1024, 256, "norm_xattn1"), ("xa_w_kv1", 1024, 512, "norm_mem1"), ("xa_w_o1", 256, 1024, None),
    ("ff_w_gu", 1024, 5632, "norm_ffn0"), ("ff_w_down", 2816, 1024, None),
    ("moe_router", 1024, 8, "norm_ffn1"),
] + [(f"moe_w_gu{e}", 1024, 2816, "norm_ffn1") for e in range(8)] + [(f"moe_w_down{e}", 1408, 1024, None) for e in range(8)]


def host_weights(inp):
    w = {}
    w["ev_w_in"] = inp["ev_w_in"][0]; w["ev_w_out"] = inp["ev_w_out"][0]
    w["od_w_in"] = inp["od_w_in"][0]; w["od_w_out"] = inp["od_w_out"][0]
    for l in range(2):
        w[f"xa_w_q{l}"] = inp["xa_w_q"][l]; w[f"xa_w_kv{l}"] = inp["xa_w_kv"][l]; w[f"xa_w_o{l}"] = inp["xa_w_o"][l]
    w["ff_w_gu"] = inp["ff_w_gu"][0]; w["ff_w_down"] = inp["ff_w_down"][0]
    w["moe_router"] = inp["moe_router"][0]
    for e in range(8):
        w[f"moe_w_gu{e}"] = inp["moe_w_gu"][0, e]; w[f"moe_w_down{e}"] = inp["moe_w_down"][0, e]
    return {k: np.ascontiguousarray(v, dtype=np.float32) for k, v in w.items()}


def ssl(start, n, step):
    return slice(start, start + (n - 1) * step + 1, step)


class Prog:
    def __init__(self, nseq, stop="end"):
        self.nseq, self.stop = nseq, stop
        self.dbgs = []
        self.nc = nc = bass.Bass("TRN2", target_bir_lowering=False)
        self.es = contextlib.ExitStack()
        NT = nseq * S
        ext = lambda n, shp, kind: V(nc.dram_tensor(n, list(shp), F32, kind=kind).ap(), [Buf(n)])
        self.x = ext("x", [NT, D], "ExternalInput")
        self.mem = ext("mem", [nseq * NMEM, D], "ExternalInput")
        self.cst_d = ext("cst", [128, NCST], "ExternalInput")
        self.w32 = {n: ext(n, [r, c], "ExternalInput") for n, r, c, g in WEIGHTS}
        self.out = ext("out", [NT, D], "ExternalOutput")

    def build(self):
        with self.es:
            self.k = K(self.nc, self.es)
            self._build()
            self.k.barrier()
        return self.nc

    def _build(self):
        k, nc, nseq = self.k, self.nc, self.nseq
        NT = nseq * S
        idr = k.dram
        self.wb = {n: idr("wb_" + n, [r, c], BF16) for n, r, c, g in WEIGHTS}
        self.hA = idr("hA", [NT, D], F32)
        self.hB = idr("hB", [NT, D], F32)
        self.att = idr("att", [3, NT, 520], F32)
        self.ogd = idr("ogd", [nseq, 1024, S], BF16)
        self.hbuf = {id(t): [Buf() for _ in range(NT // 128)] for t in (self.x, self.hA, self.hB, self.out)}
        self.cst = k.sb([128, NCST], F32, "cst")
        k.dma(self.cst, self.cst_d)
        self.cb = {}
        for n in ("ident", "bones64", "ones", "mask"):
            o, w = CST[n]
            t = k.sb([128, w], BF16, "c_" + n)
            k.copy(t, self.cst[:, o:o + w])
            self.cb[n] = t
        self.dnc = {}
        for n in ("tri_incl", "blk64", "gtk"):
            o, w = CST[n]
            t_ = k.sb([128, w], BF16, "c_" + n)
            k.copy(t_, self.cst[:, o:o + w])
            self.dnc[n] = t_
        self.eps = k.sb([128, 1], F32, "eps")
        k.memset(self.eps, EPS)
        self.gq8 = k.sb([128, 4], F32, "gq8")
        k.activation(self.gq8[:, 0:1], self.c("ev_qn"), AF.Copy, scale=0.125)
        k.activation(self.gq8[:, 1:2], self.c("xa_qn0"), AF.Copy, scale=0.125)
        k.activation(self.gq8[:, 2:3], self.c("xa_qn1"), AF.Copy, scale=0.125)

        if self.stop == "scantest":
            NTT = 4
            tq = V(nc.dram_tensor("tq", [128, 24, NTT * 128], F32, kind="ExternalInput").ap(), [Buf()])
            tbg = V(nc.dram_tensor("tbg", [128, NTT, 16], F32, kind="ExternalInput").ap(), [Buf()])
            tog = V(nc.dram_tensor("tog", [128, NTT, 8, 128], F32, kind="ExternalOutput").ap(), [Buf()])
            with k.scope() as es:
                q32 = k.sb([128, 24, NTT * 128], F32, "q32", es)
                qkvT = k.sb([128, 24, NTT * 128], BF16, "qkvT", es)
                bg = k.sb([128, NTT, 16], F32, "bg", es)
                ogf = k.sb([128, NTT, 8, 128], F32, "ogf", es)
                k.dma(q32, tq)
                k.dma(bg, tbg)
                k.copy(qkvT, q32)
                self.dn_scan(qkvT, bg, NTT, lambda t, og: k.copy(ogf[:, t, :, :], og, e=k.pool))
                k.dma(tog, ogf)
            return
        if self.layer0() == "stop":
            return
        for s in range(nseq):
            self.deltanet(s, self.hB, self.hA)
            k.barrier()
        if self.stop.startswith("mix1"):
            return self.copy_out(self.hA)
        for s in range(nseq):
            self.xattn(s, 1, self.hA, self.hB)
            k.barrier()
        if self.stop == "xa1":
            return self.copy_out(self.hB)
        for s in range(nseq):
            self.moe(s, self.hB, self.out)
            k.barrier()

    def layer0(self):
        k, nseq = self.k, self.nseq
        with k.scope() as esbg:
            L0W = ("ev_w_in", "ev_w_out")
            self.bg32 = [k.sb([128, 1024], F32, "bg32", esbg) for _ in range(3)]
            self.bgb = [k.sb([128, 1024], BF16, "bgb", esbg) for _ in range(3)]
            self.phase_convert(L0W)
            k.barrier()
            if self.stop == "convert":
                return "stop"
            order = ["xa_w_q0", "xa_w_kv0", "xa_w_o0", "ff_w_gu", "ff_w_down"]
            order += [n for n, r, c, g in WEIGHTS if n not in L0W and n not in order]
            self.bg_done = set()
            self.bg = self.bg_convert_gen(order)
            for s in range(nseq):
                self.l0_mixer(s)
                k.barrier()
            if self.stop.startswith("mix0"):
                self.copy_out(self.hB)
                return "stop"
            self.drain_until(("xa_w_q0", "xa_w_kv0", "xa_w_o0"))
            for s in range(nseq):
                self.xattn(s, 0, self.hB, self.hA)
                k.barrier()
            if self.stop == "xa0":
                self.copy_out(self.hA)
                return "stop"
            last = self.stop == "ffn0"
            self.drain_until(("ff_w_gu", "ff_w_down"))
            for s in range(nseq):
                self.ffn(s, self.hA, self.out if last else self.hB)
                k.barrier()
            if last:
                return "stop"
            for _ in self.bg:
                pass
            k.barrier()
        return "ok"

    def dbg(self, name, v, es):
        if not (self.stop.endswith("dbg") or self.stop == "scantest"):
            return
        import os
        sel = os.environ.get("DBGSEL")
        if sel is not None and name not in sel.split(","):
            return
        k = self.k
        shp = list(v.shape)
        t32 = k.sb(shp, F32, "dbg", es)
        k.copy(t32, v)
        d = V(self.nc.dram_tensor("dbg_" + name, shp, F32, kind="ExternalOutput").ap(), [Buf()])
        k.dma(d, t32)
        self.dbgs.append("dbg_" + name)

    def c(self, name):
        o, w = CST[name]
        return self.cst[:, o:o + w]

    def rows(self, t, s, i, n=1):
        g = s * (S // 128) + i
        return t[g * 128:(g + n) * 128, :].on(*self.hbuf[id(t)][g:g + n])

    def copy_out(self, src):
        k = self.k
        with k.scope() as es:
            bufs = [k.sb([128, D], F32, "co", es) for _ in range(3)]
            for s in range(self.nseq):
                for i in range(S // 128):
                    b = bufs[i % 3]
                    k.dma(b, self.rows(src, s, i))
                    k.dma(self.rows(self.out, s, i), b, q=k.pool)
        k.barrier()

    def phase_convert(self, names):
        k = self.k
        CBLK = 2048
        with k.scope() as es:
            st32 = [k.sb([128, CBLK], F32, "st32", es) for _ in range(3)]
            stb = [k.sb([128, CBLK], BF16, "stb", es) for _ in range(3)]
            it = 0
            engs = [k.dve, k.act, k.dve]
            for n, r, cdim, g in WEIGHTS:
                if n not in names:
                    continue
                for kc in range(r // 128):
                    for c0 in range(0, cdim, CBLK):
                        cbk = min(CBLK, cdim - c0)
                        a, b, e = st32[it % 3], stb[it % 3], engs[it % 3]
                        it += 1
                        k.dma(a[:, :cbk], self.w32[n][kc * 128:(kc + 1) * 128, c0:c0 + cbk])
                        if g is None:
                            k.copy(b[:, :cbk], a[:, :cbk], e=e)
                        else:
                            gcol = self.c(g)[:, kc:kc + 1]
                            if e is k.act:
                                k.activation(b[:, :cbk], a[:, :cbk], AF.Identity, scale=gcol)
                            else:
                                k.ts(b[:, :cbk], a[:, :cbk], gcol, ALU.mult, e=e)
                        k.dma(self.wb[n][kc * 128:(kc + 1) * 128, c0:c0 + cbk], b[:, :cbk], q=k.act)

    def bg_convert_gen(self, names):
        k = self.k
        CB = 1024
        NB = len(self.bg32)
        blocks = []
        wd_ = {n: (r, cdim, g) for n, r, cdim, g in WEIGHTS}
        for n in names:
            r, cdim, g = wd_[n]
            for kc in range(r // 128):
                for c0 in range(0, cdim, CB):
                    blocks.append((n, kc, c0, min(CB, cdim - c0), g))
        last_of = {blk[0]: i for i, blk in enumerate(blocks)}

        def finish(i):
            n, kc, c0, cbk, g = blocks[i]
            if last_of[n] == i:
                self.bg_done.add(n)
            a, b = self.bg32[i % NB], self.bgb[i % NB]
            if g is None:
                k.copy(b[:, :cbk], a[:, :cbk], e=k.act)
            else:
                k.activation(b[:, :cbk], a[:, :cbk], AF.Identity, scale=self.c(g)[:, kc:kc + 1])
            k.dma(self.wb[n][kc * 128:(kc + 1) * 128, c0:c0 + cbk], b[:, :cbk], q=k.act)
        for i, (n, kc, c0, cbk, g) in enumerate(blocks):
            k.dma(self.bg32[i % NB][:, :cbk], self.w32[n][kc * 128:(kc + 1) * 128, c0:c0 + cbk])
            if i > 1:
                finish(i - 2)
            yield
        for i in range(max(0, len(blocks) - 2), len(blocks)):
            finish(i)

    def drain_until(self, names):
        while not set(names) <= self.bg_done:
            if next(self.bg, "done") == "done":
                break
        self.k.barrier()

    def pump(self, n=1):
        for _ in range(n):
            if next(self.bg, "done") == "done":
                break

    def norm_T(self, es, src, s, uT, ub, ntiles=S // 128, tile0=0, rows_fn=None):
        k = self.k
        if isinstance(es, dict):
            nb = es
        else:
            nb = self.norm_bufs(es, ntiles)
        hts, xns, junk, st = nb["hts"], nb["xns"], nb["junk"], nb["st"]
        for t in range(ntiles):
            ht, xn = hts[t % len(hts)], xns[t % 2]
            k.dma(ht, rows_fn(t) if rows_fn else self.rows(src, s, tile0 + t))
            ss, rs, rstd = st[:, 3 * t:3 * t + 1], st[:, 3 * t + 1:3 * t + 2], st[:, 3 * t + 2:3 * t + 3]
            k.activation(junk, ht, AF.Square)
            k.reduce(ss, junk, ALU.add)
            k.activation(rs, ss, AF.Sqrt, scale=1.0 / D, bias=self.eps)
            k.recip(rstd, rs)
            k.ts(xn, ht, rstd, ALU.mult)
            ps = k.psum()
            psb = ps.bitcast(BF16)
            for c in range(8):
                k.tr(psb[:, c * 128:(c + 1) * 128], xn[:, c * 128:(c + 1) * 128], self.cb["ident"])
            k.copy(uT[:, :, t * 128:(t + 1) * 128].on(ub[t]), psb.rearrange("p (c t) -> p c t", c=8),
                   e=k.act if t % 2 else k.dve)

    def norm_bufs(self, es, ntiles=S // 128, nh=3):
        k = self.k
        return {"hts": [k.sb([128, D], F32, "nt_h", es) for _ in range(nh)],
                "xns": [k.sb([128, D], BF16, "nt_x", es) for _ in range(2)],
                "junk": k.sb([128, D], F32, "nt_j", es),
                "st": k.sb([128, 3 * ntiles], F32, "nt_s", es)}

    def wload(self, dst, name, c0, cw, kc0=0, nkc=None):
        k = self.k
        w = self.wb[name]
        nkc = nkc if nkc is not None else w.shape[0] // 128
        src = w[kc0 * 128:(kc0 + nkc) * 128, c0:c0 + cw].rearrange("(kc p) c -> p kc c", p=128)
        k.dma(dst, src)

    def headnorm(self, ps, out, gcol, ones_name, inv_dim, pool, n):
        k = self.k
        sq, r = pool["sq"][n % 2], pool["r"][n % 2]
        w = ps.shape[-1]
        k.activation(sq[:, :w], ps, AF.Square)
        ps2 = k.psum()
        k.mm(ps2[:, :w], self.cb[ones_name], sq[:, :w])
        k.activation(r[:, :w], ps2[:, :w], AF.Ln, scale=inv_dim, bias=self.eps)
        k.activation(r[:, :w], r[:, :w], AF.Exp, scale=-0.5)
        k.stt(out, ps, gcol, r[:, :w], ALU.mult, ALU.mult)

    def l0_mixer(self, s):
        k = self.k
        NTL = S // 128
        with k.scope() as es0:
            aT = k.sb([128, 4, S], BF16, "aT", es0)
            with k.scope() as es1:
                qT = k.sb([128, 4, S], BF16, "qT", es1)
                kT = k.sb([128, 4, S], BF16, "kT", es1)
                uT = k.sb([128, 8, S], BF16, "uT", es1)
                ub = [Buf() for _ in range(NTL)]
                wbl = [k.sb([128, 8, 512], BF16, "wbl", es1) for _ in range(3)]
                self.norm_T(self.norm_bufs(es1, nh=2), self.x, s, uT, ub)
                self.dbg("uT", uT[:, :, 0:256], es1)
                for i in range(3):
                    self.wload(wbl[i], "ev_w_in", i * 512, 512)
                with k.scope() as es:
                    ys = [k.sb([128, S + 2], F32, "y", es) for _ in range(2)]
                    gbs = [k.sb([128, S], BF16, "gbs", es) for _ in range(2)]
                    tmp = [k.sb([128, 512], F32, "tgc", es) for _ in range(2)]
                    acc = [k.sb([128, S], F32, "acc", es) for _ in range(2)]
                    for y in ys:
                        k.memset(y[:, 0:2], 0.0)
                    cw = self.c("ev_conv")
                    for c in range(4):
                        y, gb = ys[c % 2], gbs[c % 2]
                        for n in range(4):
                            pss = [k.psum() for _ in range(3)]
                            for j in range(3):
                                for kc in range(8):
                                    k.mm(pss[j], wbl[j][:, kc, c * 128:(c + 1) * 128],
                                         uT[:, kc, n * 512:(n + 1) * 512].on(*ub[4 * n:4 * n + 4]), start=kc == 0, stop=kc == 7)
                            t_ = tmp[n % 2]
                            k.activation(t_, pss[1], AF.Copy)
                            k.tt(y[:, 2 + n * 512:2 + (n + 1) * 512], t_, pss[2], ALU.mult)
                            k.activation(gb[:, n * 512:(n + 1) * 512], pss[0], AF.Copy)
                            self.pump()
                        a_ = acc[c % 2]
                        k.ts(a_, y[:, 2:S + 2], cw[:, c * 3 + 2:c * 3 + 3], ALU.mult)
                        k.stt(a_, y[:, 1:S + 1], cw[:, c * 3 + 1:c * 3 + 2], a_, ALU.mult, ALU.add)
                        k.stt(a_, y[:, 0:S], cw[:, c * 3 + 0:c * 3 + 1], a_, ALU.mult, ALU.add)
                        k.tt(aT[:, c, :], a_, gb, ALU.mult, e=k.pool)
                for i in range(3):
                    self.wload(wbl[i], "ev_w_in", 1536 + i * 512, 512)
                with k.scope() as es:
                    pool = {"sq": [k.sb([128, 512], BF16, "sq", es) for _ in range(2)],
                            "r": [k.sb([128, 512], F32, "r", es) for _ in range(2)]}
                    it = 0
                    for j, (XT, g) in enumerate(((qT, self.gq8[:, 0:1]), (kT, self.c("ev_kn")))):
                        for c in range(4):
                            for n in range(4):
                                ps = k.psum()
                                for kc in range(8):
                                    k.mm(ps, wbl[j][:, kc, c * 128:(c + 1) * 128],
                                         uT[:, kc, n * 512:(n + 1) * 512].on(*ub[4 * n:4 * n + 4]), start=kc == 0, stop=kc == 7)
                                self.headnorm(ps, XT[:, c, n * 512:(n + 1) * 512], g, "bones64", 1.0 / 64, pool, it)
                                it += 1
                                self.pump()
                self.dbg("aT", aT[:, :, 0:256], es1)
                self.dbg("qT", qT[:, :, 0:256], es1)
                self.dbg("kT", kT[:, :, 0:256], es1)
                with k.scope() as es:
                    vA = k.sb([128, 16, 8, 65], BF16, "vA", es)
                    k.memset(vA[:, :, :, 64:65], 1.0, e=k.pool)
                    P = [[k.sb([128, 2, 256], BF16, "P", es) for _ in range(3)] for _ in range(4)]
                    osb = [k.sb([128, 8, 65], F32, "osb", es) for _ in range(2)]
                    m512 = k.sb([128, 2, 256], BF16, "m512", es)
                    k.copy(m512[:, 0, :], self.cb["mask"])
                    k.copy(m512[:, 1, :], self.cb["mask"])
                    nq_it = 0
                    for pi, d in enumerate((1, 4, 16)):
                        nb = 16 // d
                        att = self.att[pi]
                        vb = [Buf() for _ in range(16)]
                        for r in range(d):
                            for lb in range(nb):
                                blk = r * nb + lb
                                st = lb * 128 * d + r
                                ps = k.psum()
                                tl = ub[lb * d:(lb + 1) * d]
                                for kc in range(8):
                                    k.mm(ps, uT[:, kc, ssl(st, 128, d)].on(*tl), wbl[2][:, kc, :], start=kc == 0, stop=kc == 7)
                                k.copy(vA[:, blk, :, 0:64].on(vb[blk]), ps.rearrange("p (h e) -> p h e", h=8),
                                       e=k.act if blk % 2 else k.dve)
                        if pi == 0:
                            self.dbg("vA", vA[:, 0:2, :, :].rearrange("p a h e -> p (a h e)"), es)
                        for r in range(d):
                            def scores(kb):
                                nq = 256 if kb + 1 < nb else 128
                                ks = kb * 128 * d + r
                                for g in range(4):
                                    e_, hq = g // 2, g % 2
                                    p0 = e_ * 64
                                    ps = k.psum()
                                    for i in range(2):
                                        hp = 2 * hq + i
                                        k.mm(ps[:, i * 256:i * 256 + nq], kT[p0:p0 + 64, hp, ssl(ks, 128, d)],
                                             qT[p0:p0 + 64, hp, ssl(ks, nq, d)])
                                    Pv = P[g][kb % 3][:, :, :nq]
                                    pv = ps.rearrange("p (e q) -> p e q", e=2)[:, :, :nq]
                                    k.activation(Pv, pv, AF.Exp)
                                    k.tt(Pv, Pv, m512[:, :, :nq], ALU.mult, e=k.pool if g % 2 else k.dve)
                            scores(0)
                            for kb in range(nb):
                                if kb + 1 < nb:
                                    scores(kb + 1)
                                pso = [k.psum(), k.psum()]
                                for h in range(8):
                                    hp, e_ = h // 2, h % 2
                                    g, i = e_ * 2 + hp // 2, hp % 2
                                    reg = pso[h // 4][:, (h % 4) * 65:(h % 4) * 65 + 65]
                                    if kb > 0:
                                        b0 = r * nb + kb - 1
                                        k.mm(reg, P[g][(kb - 1) % 3][:, i, 128:256], vA[:, b0, h, :].on(vb[b0], *vA.bufs), start=True, stop=False)
                                    b1 = r * nb + kb
                                    k.mm(reg, P[g][kb % 3][:, i, 0:128], vA[:, b1, h, :].on(vb[b1], *vA.bufs), start=kb == 0, stop=True)
                                ob = osb[nq_it % 2]
                                for half in range(2):
                                    k.copy(ob[:, half * 4:(half + 1) * 4, :],
                                           pso[half][:, 0:260].rearrange("p (h e) -> p h e", h=4),
                                           e=k.act if nq_it % 2 else k.dve)
                                nq_it += 1
                                base = s * S + kb * 128 * d + r
                                dst = att[ssl(base, 128, d), :]
                                k.dma(dst.on(Buf()), ob.rearrange("p h e -> p (h e)"), q=k.pool)
                                self.pump()
            k.barrier()
            with k.scope() as es:
                wout = k.sb([128, 8, D], BF16, "wout", es)
                self.wload(wout, "ev_w_out", 0, D)
                NBF = 3
                a3 = [k.sb([128, 3, 520], F32, "a3", es) for _ in range(NBF)]
                sm = [k.sb([128, 520], F32, "sm", es) for _ in range(NBF)]
                rd = [k.sb([128, 8], F32, "rd", es) for _ in range(NBF)]
                obf = [k.sb([128, 512], BF16, "obf", es) for _ in range(NBF)]
                oTt = [k.sb([128, 4, 128], BF16, "oTt", es) for _ in range(NBF)]
                hts = [k.sb([128, D], F32, "hres", es) for _ in range(NBF)]
                hos = [k.sb([128, D], F32, "hout", es) for _ in range(NBF)]

                def tile_gen(t):
                    g = s * NTL + t
                    i = t % NBF
                    a, sm_, rd_, ob_, ht, ho, oT_ = a3[i], sm[i], rd[i], obf[i], hts[i], hos[i], oTt[i]
                    src = self.att[:, g * 128:(g + 1) * 128, :].rearrange("g t c -> t g c")
                    k.dma(a, src.on(Buf()))
                    k.dma(ht, self.rows(self.x, s, t))
                    k.tt(sm_, a[:, 0, :], a[:, 1, :], ALU.add, e=k.pool)
                    k.tt(sm_, sm_, a[:, 2, :], ALU.add, e=k.pool)
                    s3 = sm_.rearrange("p (h e) -> p h e", h=8)
                    k.recip(rd_, s3[:, :, 64])
                    k.tt(ob_.rearrange("p (h e) -> p h e", h=8), s3[:, :, 0:64],
                         rd_.rearrange("p (h o) -> p h o", o=1).bc([128, 8, 64]), ALU.mult)
                    yield
                    ps = k.psum()
                    psb = ps.bitcast(BF16)
                    for c in range(4):
                        k.tr(psb[:, c * 128:(c + 1) * 128], ob_[:, c * 128:(c + 1) * 128], self.cb["ident"])
                    k.copy(oT_, psb[:, 0:512].rearrange("p (c t) -> p c t", c=4), e=k.act)
                    yield
                    for n in range(2):
                        ps = k.psum()
                        for c in range(8):
                            lhs = aT[:, c, t * 128:(t + 1) * 128] if c < 4 else oT_[:, c - 4, :]
                            k.mm(ps, lhs, wout[:, c, n * 512:(n + 1) * 512], start=c == 0, stop=c == 7)
                        k.tt(ho[:, n * 512:(n + 1) * 512], ps, ht[:, n * 512:(n + 1) * 512], ALU.add)
                    k.dma(self.rows(self.hB, s, t), ho, q=k.pool)
                    self.pump()

                active = []
                for t in range(NTL + 2):
                    for g_ in list(active):
                        if next(g_, "done") == "done":
                            active.remove(g_)
                    if t < NTL:
                        g_ = tile_gen(t)
                        next(g_)
                        active.append(g_)
                for g_ in active:
                    for _ in g_:
                        pass

    def xattn(self, s, layer, hin, hout):
        k = self.k
        NTL = S // 128
        wq, wkv, wo = f"xa_w_q{layer}", f"xa_w_kv{layer}", f"xa_w_o{layer}"
        with k.scope() as es0:
            uT = k.sb([128, 8, S], BF16, "uT", es0)
            ub = [Buf() for _ in range(NTL)]
            mT = k.sb([128, 8, NMEM], BF16, "mT", es0)
            mb = [Buf() for _ in range(2)]
            qT = k.sb([128, 2, S], BF16, "xqT", es0)
            kT = k.sb([128, 2, NMEM], BF16, "xkT", es0)
            vA = k.sb([128, 2, 4, 65], BF16, "xvA", es0)
            k.memset(vA[:, :, :, 64:65], 1.0, e=k.pool)
            wqs = k.sb([128, 8, 256], BF16, "wqs", es0)
            wkvs = k.sb([128, 8, 512], BF16, "wkvs", es0)
            wos = k.sb([128, 2, D], BF16, "wos", es0)
            self.wload(wqs, wq, 0, 256)
            self.wload(wkvs, wkv, 0, 512)
            self.wload(wos, wo, 0, D)
            nbx = self.norm_bufs(es0)
            self.norm_T(nbx, None, s, mT, mb, ntiles=2,
                        rows_fn=lambda t: self.mem[(s * 2 + t) * 128:(s * 2 + t + 1) * 128, :])
            self.norm_T(nbx, hin, s, uT, ub)
            with k.scope() as es:
                pool = {"sq": [k.sb([128, 512], BF16, "sq", es) for _ in range(2)],
                        "r": [k.sb([128, 512], F32, "r", es) for _ in range(2)]}
                it = 0
                for c in range(2):
                    ps = k.psum()
                    for kc in range(8):
                        k.mm(ps[:, :NMEM], wkvs[:, kc, c * 128:(c + 1) * 128], mT[:, kc, :].on(*mb), start=kc == 0, stop=kc == 7)
                    self.headnorm(ps[:, :NMEM], kT[:, c, :], self.c(f"xa_kn{layer}"), "bones64", 1.0 / 64, pool, it)
                    it += 1
                for mblk in range(2):
                    ps = k.psum()
                    for kc in range(8):
                        k.mm(ps[:, :256], mT[:, kc, mblk * 128:(mblk + 1) * 128].on(mb[mblk]), wkvs[:, kc, 256:512], start=kc == 0, stop=kc == 7)
                    k.copy(vA[:, mblk, :, 0:64], ps[:, :256].rearrange("p (h e) -> p h e", h=4))
                for c in range(2):
                    for n in range(4):
                        ps = k.psum()
                        for kc in range(8):
                            k.mm(ps, wqs[:, kc, c * 128:(c + 1) * 128], uT[:, kc, n * 512:(n + 1) * 512].on(*ub[4 * n:4 * n + 4]),
                                 start=kc == 0, stop=kc == 7)
                        self.headnorm(ps, qT[:, c, n * 512:(n + 1) * 512], self.gq8[:, 1 + layer:2 + layer], "bones64", 1.0 / 64, pool, it)
                        it += 1
            with k.scope() as es:
                Pt = [[k.sb([128, 2, 512], BF16, "xP", es) for _ in range(2)] for _ in range(4)]
                NBF = 3
                osb = [k.sb([128, 4, 65], F32, "xo", es) for _ in range(NBF)]
                rd = [k.sb([128, 4], F32, "xrd", es) for _ in range(NBF)]
                obf = [k.sb([128, 256], BF16, "xob", es) for _ in range(NBF)]
                oTt = [k.sb([128, 2, 128], BF16, "xoT", es) for _ in range(NBF)]
                hts = [k.sb([128, D], F32, "hres", es) for _ in range(NBF)]
                hos = [k.sb([128, D], F32, "hout", es) for _ in range(NBF)]

                def scores(n):
                    for h in range(4):
                        c, p0 = h // 2, (h % 2) * 64
                        for mblk in range(2):
                            ps = k.psum()
                            k.mm(ps, kT[p0:p0 + 64, c, mblk * 128:(mblk + 1) * 128], qT[p0:p0 + 64, c, n * 512:(n + 1) * 512])
                            k.activation(Pt[h][n % 2][:, mblk, :], ps, AF.Exp)

                def tile_gen(t):
                    n, tt_ = t // 4, t % 4
                    i = t % NBF
                    ht, ho = hts[i], hos[i]
                    k.dma(ht, self.rows(hin, s, t))
                    pso = k.psum()
                    for h in range(4):
                        reg = pso[:, h * 65:(h + 1) * 65]
                        for mblk in range(2):
                            k.mm(reg, Pt[h][n % 2][:, mblk, tt_ * 128:(tt_ + 1) * 128], vA[:, mblk, h, :], start=mblk == 0, stop=mblk == 1)
                    o_ = osb[i]
                    k.copy(o_, pso[:, 0:260].rearrange("p (h e) -> p h e", h=4), e=k.act)
                    k.recip(rd[i], o_[:, :, 64])
                    k.tt(obf[i].rearrange("p (h e) -> p h e", h=4), o_[:, :, 0:64],
                         rd[i].rearrange("p (h o) -> p h o", o=1).bc([128, 4, 64]), ALU.mult)
                    yield
                    ps = k.psum()
                    psb = ps.bitcast(BF16)
                    for c in range(2):
                        k.tr(psb[:, c * 128:(c + 1) * 128], obf[i][:, c * 128:(c + 1) * 128], self.cb["ident"])
                    k.copy(oTt[i], psb[:, 0:256].rearrange("p (c t) -> p c t", c=2), e=k.act)
                    yield
                    for nn in range(2):
                        ps = k.psum()
                        for c in range(2):
                            k.mm(ps, oTt[i][:, c, :], wos[:, c, nn * 512:(nn + 1) * 512], start=c == 0, stop=c == 1)
                        k.tt(ho[:, nn * 512:(nn + 1) * 512], ps, ht[:, nn * 512:(nn + 1) * 512], ALU.add)
                    k.dma(self.rows(hout, s, t), ho, q=k.pool)
                    self.pump()

                active = []
                for t in range(NTL + 2):
                    if t < NTL and t % 4 == 0:
                        scores(t // 4)
                    for g_ in list(active):
                        if next(g_, "done") == "done":
                            active.remove(g_)
                    if t < NTL:
                        g_ = tile_gen(t)
                        next(g_)
                        active.append(g_)
                for g_ in active:
                    for _ in g_:
                        pass

    def ffn(self, s, hin, hout):
        k = self.k
        TB = 1024
        NTB = TB // 128
        NJ = DFF // 128
        with k.scope() as es0:
            wd = k.sb([128, NJ, D], BF16, "wd", es0)
            self.wload(wd, "ff_w_down", 0, D)
            uT = k.sb([128, 8, TB], BF16, "uT", es0)
            hT = k.sb([128, NJ, TB], BF16, "hT", es0)
            wg = [k.sb([128, 8, 256], BF16, "wg", es0) for _ in range(2)]
            wu = [k.sb([128, 8, 256], BF16, "wu", es0) for _ in range(2)]
            sl = [k.sb([128, 512], F32, "sl", es0) for _ in range(2)]
            hts = [k.sb([128, D], F32, "hres", es0) for _ in range(2)]
            hos = [k.sb([128, D], F32, "hout", es0) for _ in range(2)]
            nbf = self.norm_bufs(es0, NTB, nh=2)
            for blk in range(S // TB):
                ub = [Buf() for _ in range(NTB)]
                hb = [[Buf() for _ in range(2)] for _ in range(NJ)]
                self.norm_T(nbf, hin, s, uT, ub, ntiles=NTB, tile0=blk * NTB)
                it = 0
                for jg in range(NJ // 2):
                    self.wload(wg[jg % 2], "ff_w_gu", jg * 256, 256)
                    self.wload(wu[jg % 2], "ff_w_gu", DFF + jg * 256, 256)
                    for jj in range(2):
                        j = jg * 2 + jj
                        for n in range(TB // 512):
                            pg, pu = k.psum(), k.psum()
                            for ps, w in ((pg, wg[jg % 2]), (pu, wu[jg % 2])):
                                for kc in range(8):
                                    k.mm(ps, w[:, kc, jj * 128:(jj + 1) * 128], uT[:, kc, n * 512:(n + 1) * 512].on(*ub[4 * n:4 * n + 4]),
                                         start=kc == 0, stop=kc == 7)
                            s_ = sl[it % 2]
                            it += 1
                            k.activation(s_, pg, AF.Silu)
                            k.tt(hT[:, j, n * 512:(n + 1) * 512].on(hb[j][n]), s_, pu, ALU.mult)
                            self.pump()
                for t in range(NTB):
                    gt = blk * NTB + t
                    ht, ho = hts[t % 2], hos[t % 2]
                    k.dma(ht, self.rows(hin, s, gt))
                    for nn in range(2):
                        ps = k.psum()
                        for j in range(NJ):
                            k.mm(ps, hT[:, j, t * 128:(t + 1) * 128].on(hb[j][t // 4]), wd[:, j, nn * 512:(nn + 1) * 512], start=j == 0, stop=j == NJ - 1)
                        k.tt(ho[:, nn * 512:(nn + 1) * 512], ps, ht[:, nn * 512:(nn + 1) * 512], ALU.add)
                    k.dma(self.rows(hout, s, gt), ho, q=k.pool)
                    self.pump()


    def deltanet(self, s, hin, hout):
        k = self.k
        NTL = S // 128
        cf = {n: self.c(n) for n in ("tri_incl", "blk64", "strict_ij", "causal_ji", "gtk", "ones", "ident")}
        import os
        DN = int(os.environ.get("DN_STOP", "99"))
        with k.scope() as es0:
            qkvT = k.sb([128, 24, S], BF16, "qkvT", es0)
            bg = k.sb([128, NTL, 16], F32, "bg", es0)
            nexpa = k.sb([128, 8], F32, "nexpa", es0)
            one1 = k.sb([128, 1], F32, "one1", es0)
            k.memset(one1, 1.0)
            k.activation(nexpa, self.c("od_alog"), AF.Exp)
            k.ts(nexpa, nexpa, -1.0, ALU.mult)
            with k.scope() as es1:
                uT = k.sb([128, 8, S], BF16, "uT", es1)
                ub = [Buf() for _ in range(NTL)]
                with k.scope() as es:
                    self.norm_T(es, hin, s, uT, ub)
                if DN <= -1:
                    return
                wba = k.sb([128, 8, 16], BF16, "wba", es1)
                self.wload(wba, "od_w_in", 4096, 16)
                sm = [k.sb([128, 40], F32, "bgt", es1) for _ in range(2)]
                for t in range(NTL):
                    ps = k.psum()
                    for kc in range(8):
                        k.mm(ps[:, 0:16], uT[:, kc, t * 128:(t + 1) * 128].on(ub[t]), wba[:, kc, :], start=kc == 0, stop=kc == 7)
                    w = sm[t % 2]
                    k.activation(bg[:, t, 0:8], ps[:, 0:8], AF.Sigmoid)
                    k.tt(w[:, 0:8], ps[:, 8:16], self.c("od_dtb"), ALU.add)
                    k.ts(w[:, 32:40], w[:, 0:8], -1.0, ALU.mult)
                    k.tt(w[:, 8:16], w[:, 0:8], w[:, 32:40], ALU.max)
                    k.activation(w[:, 16:24], w[:, 8:16], AF.Exp, scale=-1.0)
                    k.activation(w[:, 24:32], w[:, 16:24], AF.Ln, bias=one1)
                    k.ts(w[:, 0:8], w[:, 0:8], 0.0, ALU.max)
                    k.tt(w[:, 0:8], w[:, 0:8], w[:, 24:32], ALU.add)
                    k.tt(bg[:, t, 8:16], w[:, 0:8], nexpa, ALU.mult)
                if DN <= 0:
                    return
                wch = [k.sb([128, 8, 512], BF16, "wch", es1) for _ in range(2)]
                ys = [k.sb([128, S + 3], F32, "y", es1) for _ in range(2)]
                acc = [k.sb([128, S], F32, "acc", es1) for _ in range(2)]
                sq = [k.sb([128, 512], BF16, "sq", es1) for _ in range(2)]
                rr = [k.sb([128, 512], F32, "rr", es1) for _ in range(2)]
                for y in ys:
                    k.memset(y[:, 0:3], 0.0)
                cw = self.c("od_conv")
                it = 0
                for c in [int(x) for x in os.environ.get('DN_C', ','.join(map(str, range(24)))).split(',')]:
                    wc, y, a_ = wch[(c // 4) % 2][:, :, (c % 4) * 128:(c % 4 + 1) * 128], ys[c % 2], acc[c % 2]
                    if c % 4 == 0:
                        self.wload(wch[(c // 4) % 2], "od_w_in", c * 128, 512)
                    for n in range(4):
                        ps = k.psum()
                        for kc in range(8):
                            k.mm(ps, wc[:, kc, :], uT[:, kc, n * 512:(n + 1) * 512].on(*ub[4 * n:4 * n + 4]), start=kc == 0, stop=kc == 7)
                        k.copy(y[:, 3 + n * 512:3 + (n + 1) * 512], ps, e=k.act)
                    k.ts(a_, y[:, 3:S + 3], cw[:, c * 4 + 3:c * 4 + 4], ALU.mult)
                    for j in (2, 1, 0):
                        k.stt(a_, y[:, j:S + j], cw[:, c * 4 + j:c * 4 + j + 1], a_, ALU.mult, ALU.add)
                    if c >= 16:
                        k.activation(qkvT[:, c, :], a_, AF.Silu)
                    else:
                        k.activation(a_, a_, AF.Silu)
                        for n in range(4):
                            sl = slice(n * 512, (n + 1) * 512)
                            q_, r_ = sq[it % 2], rr[it % 2]
                            it += 1
                            k.activation(q_, a_[:, sl], AF.Square)
                            ps2 = k.psum()
                            k.mm(ps2, self.cb["ones"], q_)
                            k.activation(r_, ps2, AF.Ln, bias=self.eps)
                            k.activation(r_, r_, AF.Exp, scale=-0.5)
                            if c < 8:
                                k.stt(qkvT[:, c, sl], a_[:, sl], 128.0 ** -0.5, r_, ALU.mult, ALU.mult)
                            else:
                                k.tt(qkvT[:, c, sl], a_[:, sl], r_, ALU.mult)
            def emit_og(t, og):
                tk = slice(t * 128, (t + 1) * 128)
                k.dma(self.ogd[s][:, tk].rearrange("(h p) t -> p h t", p=128).on(Buf()), og, q=k.pool)
            self.dn_scan(qkvT, bg, NTL, emit_og)
        with k.scope() as es0:
            uT = k.sb([128, 8, S], BF16, "uT", es0)
            ub = [Buf() for _ in range(NTL)]
            self.norm_T(self.norm_bufs(es0, nh=2), hin, s, uT, ub)
            ogT = k.sb([128, 8, S], BF16, "ogT", es0)
            k.dma(ogT, self.ogd[s].rearrange("(h p) t -> p h t", p=128))
            wgt = [k.sb([128, 8, 512], BF16, "wgt", es0) for _ in range(2)]
            sg = [k.sb([128, 512], F32, "sg", es0) for _ in range(2)]
            it = 0
            for c in range(8):
                if c % 4 == 0:
                    self.wload(wgt[c // 4], "od_w_in", 3072 + c * 128, 512)
                for n in range(4):
                    ps = k.psum()
                    for kc in range(8):
                        k.mm(ps, wgt[c // 4][:, kc, (c % 4) * 128:(c % 4 + 1) * 128], uT[:, kc, n * 512:(n + 1) * 512].on(*ub[4 * n:4 * n + 4]), start=kc == 0, stop=kc == 7)
                    g_ = sg[it % 2]
                    it += 1
                    k.activation(g_, ps, AF.Silu)
                    k.tt(ogT[:, c, n * 512:(n + 1) * 512], ogT[:, c, n * 512:(n + 1) * 512], g_, ALU.mult)
            wout = k.sb([128, 8, D], BF16, "wout", es0)
            self.wload(wout, "od_w_out", 0, D)
            hts = [k.sb([128, D], F32, "hres", es0) for _ in range(2)]
            hos = [k.sb([128, D], F32, "hout", es0) for _ in range(2)]
            for t in range(NTL):
                ht, ho = hts[t % 2], hos[t % 2]
                k.dma(ht, self.rows(hin, s, t))
                for n in range(2):
                    ps = k.psum()
                    for c in range(8):
                        k.mm(ps, ogT[:, c, t * 128:(t + 1) * 128], wout[:, c, n * 512:(n + 1) * 512], start=c == 0, stop=c == 7)
                    k.tt(ho[:, n * 512:(n + 1) * 512], ps, ht[:, n * 512:(n + 1) * 512], ALU.add)
                k.dma(self.rows(hout, s, t), ho, q=k.pool)

    def dn_scan(self, qkvT, bg, ntiles, emit_og):
        import os
        SS = int(os.environ.get('SCAN_STOP', '99'))
        k = self.k
        tri_b, blk_b, gtk_b = self.dnc["tri_incl"], self.dnc["blk64"], self.dnc["gtk"]
        ones_b, ident_b = self.cb["ones"], self.cb["ident"]
        tri32, strict32, causal32 = self.c("tri_incl"), self.c("strict_ij"), self.c("causal_ji")
        v3 = lambda x: x.rearrange("p (h o) -> p h o", o=1)
        r3 = lambda x: x.rearrange("p (o c) -> p o c", o=1)
        with k.scope() as es1:
            S32 = k.sb([128, 8, 128], F32, "S32", es1)
            Sbf = k.sb([128, 8, 128], BF16, "Sbf", es1)
            Sb = [Buf() for _ in range(8)]
            Sbb = [Buf() for _ in range(8)]
            k.memset(S32, 0.0)
            k.memset(Sbf, 0.0)
            mk = lambda shp, dt, nm, n=2: [k.sb(shp, dt, nm, es1) for _ in range(n)]
            mk2 = lambda shp, dt, nm, n=2: [mk(shp, dt, nm, n) for _ in range(2)]
            tsm = mk([128, 64], F32, "tsm")
            gsp = mk([128, 16], BF16, "gsp")
            Tgh = mk2([128, 4, 128], BF16, "Tgh")
            Tgl = mk2([128, 4, 128], BF16, "Tgl")
            E1 = mk2([128, 4, 128], F32, "E1", 1)
            E2 = mk2([128, 4, 128], F32, "E2", 1)
            EG = mk2([128, 4, 128], F32, "EG")
            kb_ = mk2([128, 4, 128], BF16, "kb", 1)
            kd = mk2([128, 4, 128], BF16, "kd")
            vb = mk2([128, 4, 128], BF16, "vb", 1)
            qd = mk2([128, 4, 128], BF16, "qd")
            attT = mk2([128, 4, 128], BF16, "attT")
            X = mk2([128, 4, 384], BF16, "X", 1)
            Xb = [Buf() for _ in range(8)]
            uu = mk2([128, 4, 128], F32, "uu")
            wT = mk2([128, 4, 128], BF16, "wT")
            vn = mk([128, 128], BF16, "vn", 4)
            sqo = mk([128, 128], BF16, "sqo")
            ro = mk([128, 128], F32, "ro")
            ogs = mk([128, 8, 128], BF16, "ogs")

            def pre(t, hf):
                tk = slice(t * 128, (t + 1) * 128)
                p2_ = t % 2
                H0 = hf * 4
                w, gs = tsm[p2_], gsp[p2_]
                beta, g = bg[:, t, 0:8], bg[:, t, 8:16]
                if hf == 0:
                    k.copy(gs[:, 0:8], g)
                    k.copy(w[:, 32:40], gs[:, 0:8])
                    k.tt(w[:, 40:48], g, w[:, 32:40], ALU.subtract)
                    k.copy(gs[:, 8:16], w[:, 40:48])
                    ps = k.psum()
                    k.mm(ps[:, 0:8], tri_b, gs[:, 0:8], start=True, stop=False)
                    k.mm(ps[:, 0:8], tri_b, gs[:, 8:16], start=False, stop=True)
                    k.mm(ps[:, 8:16], blk_b, gs[:, 0:8], start=True, stop=False)
                    k.mm(ps[:, 8:16], blk_b, gs[:, 8:16], start=False, stop=True)
                    k.activation(w[:, 0:8], ps[:, 0:8], AF.Exp)
                    k.copy(w[:, 8:16], ps[:, 0:8])
                    k.tt(w[:, 16:24], ps[:, 8:16], w[:, 8:16], ALU.subtract)
                    k.activation(w[:, 16:24], w[:, 16:24], AF.Exp)
                    k.tt(w[:, 24:32], w[:, 0:8], beta, ALU.mult)
                yield
                wh = lambda c0: w[:, c0 + H0:c0 + H0 + 4]
                bh = beta[:, H0:H0 + 4]
                th, tl = Tgh[hf][p2_], Tgl[hf][p2_]
                k.tt(th, r3(tri32).bc([128, 4, 128]), v3(wh(32)).bc([128, 4, 128]), ALU.mult)
                k.tt(tl, r3(tri32).bc([128, 4, 128]), v3(wh(40)).bc([128, 4, 128]), ALU.mult, e=k.pool)
                yield
                e1, e2, eg = E1[hf][0], E2[hf][0], EG[hf][p2_]
                f2 = lambda v_: v_.rearrange("p h c -> p (h c)")
                pa = k.psum()
                k.mm(pa, gtk_b, f2(th), start=True, stop=False)
                k.mm(pa, gtk_b, f2(tl), start=False, stop=True)
                k.activation(f2(e2), pa, AF.Exp)
                pb = k.psum()
                k.mm(pb, ones_b, f2(th), start=True, stop=False)
                k.mm(pb, ones_b, f2(tl), start=False, stop=True)
                k.activation(f2(eg), pb, AF.Exp)
                yield
                pc = k.psum()
                for hh in range(4):
                    k.mm(pc[:, hh * 128:(hh + 1) * 128], th[:, hh, :], gtk_b, start=True, stop=False)
                    k.mm(pc[:, hh * 128:(hh + 1) * 128], tl[:, hh, :], gtk_b, start=False, stop=True)
                k.activation(f2(e1), pc, AF.Exp)
                yield
                k.tt(e2, e2, r3(causal32).bc([128, 4, 128]), ALU.mult, e=k.pool)
                k.tt(e1, e1, r3(strict32).bc([128, 4, 128]), ALU.mult)
                k.tt(e1, e1, v3(bh).bc([128, 4, 128]), ALU.mult)
                k.tt(qd[hf][p2_], qkvT[:, H0:H0 + 4, tk], eg, ALU.mult, e=k.pool)
                yield
                pkt = k.psum()
                pkb = pkt.bitcast(BF16)
                for hh in range(4):
                    k.tr(pkb[:, hh * 128:(hh + 1) * 128], qkvT[:, 8 + H0 + hh, tk], ident_b)
                for hh in range(4):
                    k.tr(pkb[:, 512 + hh * 128:512 + (hh + 1) * 128], qkvT[:, 16 + H0 + hh, tk], ident_b)
                pk3 = pkb[:, 0:512].rearrange("p (h c) -> p h c", h=4)
                pv3 = pkb[:, 512:1024].rearrange("p (h c) -> p h c", h=4)
                k.tt(kb_[hf][0], pk3, v3(wh(24)).bc([128, 4, 128]), ALU.mult)
                k.tt(kd[hf][p2_], pk3, v3(wh(16)).bc([128, 4, 128]), ALU.mult)
                k.tt(vb[hf][0], pv3, v3(bh).bc([128, 4, 128]), ALU.mult)
                yield
                x = X[hf][0]
                xb = Xb[H0:H0 + 4]
                for hp in range(2):
                    pk = k.psum()
                    for i in range(2):
                        h = H0 + hp * 2 + i
                        kT_, qT_ = qkvT[:, 8 + h, tk], qkvT[:, h, tk]
                        k.mm(pk[:, i * 256:i * 256 + 128], kT_, kT_)
                        k.mm(pk[:, i * 256 + 128:i * 256 + 256], kT_, qT_)
                    pk4 = pk.rearrange("p (h a c) -> p h a c", h=2, a=2)
                    hs = slice(hp * 2, hp * 2 + 2)
                    k.tt(x[:, hs, 256:384].on(xb[hp * 2], xb[hp * 2 + 1]), pk4[:, :, 0, :], e1[:, hs, :], ALU.mult)
                    k.tt(attT[hf][p2_][:, hs, :], pk4[:, :, 1, :], e2[:, hs, :], ALU.mult)
                    yield
                ptr = k.psum()
                ptb = ptr.bitcast(BF16)
                for hh in range(4):
                    k.tr(ptb[:, hh * 128:(hh + 1) * 128], x[:, hh, 256:384].on(xb[hh]), ident_b)
                pt3 = ptb[:, 0:512].rearrange("p (h c) -> p h c", h=4)
                k.copy(x[:, :, 128:256].on(*xb), pt3, e=k.act)
                k.tt(x[:, :, 0:128].on(*xb), r3(ident_b).bc([128, 4, 128]), pt3, ALU.subtract)
                yield
                for lvl in range(5):
                    for hh in range(4):
                        xh = x[:, hh, :].on(xb[hh])
                        ev = k.act if hh % 2 else k.dve
                        pl = k.psum()
                        if lvl == 0:
                            k.mm(pl[:, 128:256], xh[:, 256:384], xh[:, 128:256])
                            k.mm(pl[:, 256:384], xh[:, 128:256], xh[:, 256:384])
                            k.copy(xh[:, 128:384], pl[:, 128:384], e=ev)
                        else:
                            k.mm(pl[:, 0:256], xh[:, 256:384], xh[:, 0:256], start=True, stop=False)
                            k.mm(pl[:, 0:128], ident_b, xh[:, 0:128], start=False, stop=True)
                            k.mm(pl[:, 256:384], xh[:, 128:256], xh[:, 256:384])
                            k.copy(xh[:, 0:384], pl[:, 0:384], e=ev)
                        if hh % 2:
                            yield
                for hh in range(4):
                    xh = x[:, hh, :].on(xb[hh])
                    pl = k.psum()
                    k.mm(pl[:, 0:128], xh[:, 256:384], xh[:, 0:128], start=True, stop=False)
                    k.mm(pl[:, 0:128], ident_b, xh[:, 0:128], start=False, stop=True)
                    k.copy(xh[:, 0:128], pl[:, 0:128], e=k.dve if hh % 2 else k.act)
                    pu = k.psum()
                    k.mm(pu[:, 0:128], xh[:, 0:128], vb[hf][0][:, hh, :])
                    k.mm(pu[:, 128:256], kb_[hf][0][:, hh, :], xh[:, 0:128])
                    k.copy(uu[hf][p2_][:, hh, :], pu[:, 0:128], e=k.act)
                    k.copy(wT[hf][p2_][:, hh, :], pu[:, 128:256], e=k.act)
                    if hh % 2:
                        yield

            vit = [0]

            def scan(t):
                p2_ = t % 2
                po = [k.psum(hold=True), k.psum(hold=True)]
                for ch in range(2):
                    c0 = ch * 64
                    cs = slice(c0, c0 + 64)
                    for h in range(8):
                        hf, hh = h // 4, h % 4
                        eg = EG[hf][p2_]
                        vn_ = vn[vit[0] % 4]
                        vit[0] += 1
                        Sv = Sbf[:, h, :].on(Sbb[h])
                        p1 = k.psum()
                        k.mm(p1[cs, 0:128], wT[hf][p2_][:, hh, cs], Sv)
                        k.tt(vn_[cs, :], uu[hf][p2_][cs, hh, :], p1[cs, 0:128], ALU.subtract)
                        oreg = po[h // 4][:, (h % 4) * 128 + c0:(h % 4) * 128 + c0 + 64]
                        k.mm(oreg, Sv, qd[hf][p2_][:, hh, cs], start=True, stop=False)
                        yield
                        k.mm(oreg, vn_[cs, :], attT[hf][p2_][cs, hh, cs], start=False, stop=True)
                        p2 = k.psum()
                        k.mm(p2[:, 0:128], kd[hf][p2_][cs, hh, :], vn_[cs, :])
                        k.stt(S32[:, h, :].on(Sb[h]), S32[:, h, :].on(Sb[h]), eg[:, hh, c0 + 63:c0 + 64], p2[:, 0:128], ALU.mult, ALU.add)
                        k.copy(Sbf[:, h, :].on(Sbb[h]), S32[:, h, :].on(Sb[h]), e=k.act)
                        yield
                og = ogs[p2_]
                for h in range(8):
                    ov = po[h // 4][:, (h % 4) * 128:(h % 4 + 1) * 128]
                    q_, r_ = sqo[h % 2], ro[h % 2]
                    k.activation(q_, ov, AF.Square)
                    ps2 = k.psum()
                    k.mm(ps2[:, 0:128], ones_b, q_)
                    k.activation(r_, ps2[:, 0:128], AF.Ln, scale=1.0 / 128, bias=self.eps)
                    k.activation(r_, r_, AF.Exp, scale=-0.5)
                    k.stt(og[:, h, :], ov, self.c("od_on"), r_, ALU.mult, ALU.mult)
                    yield
                k.release(po[0])
                k.release(po[1])
                emit_og(t, og)

            def run_rr(gens):
                gens = list(gens)
                while gens:
                    for g_ in list(gens):
                        if next(g_, "done") == "done":
                            gens.remove(g_)

            g0, g1 = pre(0, 0), pre(0, 1)
            next(g0)
            run_rr([g0, g1])
            for t in range(ntiles):
                gens = [scan(t)]
                if t + 1 < ntiles:
                    g0, g1 = pre(t + 1, 0), pre(t + 1, 1)
                    next(g0)
                    gens += [g0, g1]
                run_rr(gens)

    def moe(self, s, hin, hout):
        k = self.k
        TB = 1024
        NTB = TB // 128
        NJ = DFE // 128
        with k.scope() as es0:
            uT = k.sb([128, 8, TB], BF16, "uT", es0)
            hT = k.sb([128, NJ, TB], BF16, "hT", es0)
            acc = k.sb([128, NTB, D], F32, "macc", es0)
            comb = k.sb([128, NTB, 8], F32, "comb", es0)
            wr = k.sb([128, 8, 8], BF16, "wr", es0)
            self.wload(wr, "moe_router", 0, 8)
            wd = [k.sb([128, NJ, D], BF16, "wd", es0) for _ in range(2)]
            wgh = [k.sb([128, 8, 768], BF16, "wgh", es0) for _ in range(2)]
            wuh = [k.sb([128, 8, 768], BF16, "wuh", es0) for _ in range(2)]
            sl = [k.sb([128, 512], BF16, "sl", es0) for _ in range(2)]
            rt = [k.sb([128, 48], F32, "rt", es0) for _ in range(2)]
            nbf = self.norm_bufs(es0, NTB, nh=2)
            for blk in range(S // TB):
                ub = [Buf() for _ in range(NTB)]
                ab = [Buf() for _ in range(NTB)]
                self.norm_T(nbf, hin, s, uT, ub, ntiles=NTB, tile0=blk * NTB)
                for t in range(NTB):
                    k.dma(acc[:, t, :].on(ab[t]), self.rows(hin, s, blk * NTB + t))
                    ps = k.psum()
                    for kc in range(8):
                        k.mm(ps[:, 0:8], uT[:, kc, t * 128:(t + 1) * 128].on(ub[t]), wr[:, kc, :], start=kc == 0, stop=kc == 7)
                    w = rt[t % 2]
                    k.copy(w[:, 0:8], ps[:, 0:8])
                    k.max8(w[:, 8:16], w[:, 0:8])
                    k.ts(w[:, 16:17], w[:, 8:9], -1.0, ALU.mult)
                    k.activation(w[:, 24:32], w[:, 0:8], AF.Exp, bias=w[:, 16:17])
                    k.ts(w[:, 32:40], w[:, 0:8], w[:, 9:10], ALU.is_ge)
                    k.tt(w[:, 24:32], w[:, 24:32], w[:, 32:40], ALU.mult)
                    k.reduce(w[:, 17:18], w[:, 24:32], ALU.add)
                    k.recip(w[:, 18:19], w[:, 17:18])
                    k.ts(comb[:, t, :], w[:, 24:32], w[:, 18:19], ALU.mult)
                it = 0
                HALF = ((0, 6), (6, NJ))

                def load_unit(u):
                    e_, hf = u // 2, u % 2
                    j0, j1 = HALF[hf]
                    self.wload(wgh[u % 2][:, :, 0:(j1 - j0) * 128], f"moe_w_gu{e_}", j0 * 128, (j1 - j0) * 128)
                    self.wload(wuh[u % 2][:, :, 0:(j1 - j0) * 128], f"moe_w_gu{e_}", DFE + j0 * 128, (j1 - j0) * 128)
                load_unit(0)
                self.wload(wd[0], "moe_w_down0", 0, D)
                for e in range(NEXP):
                    wde = wd[e % 2]
                    hb = [[Buf() for _ in range(2)] for _ in range(NJ)]
                    for hf in range(2):
                        u = e * 2 + hf
                        if u + 1 < 2 * NEXP:
                            load_unit(u + 1)
                        if hf == 0 and e + 1 < NEXP:
                            self.wload(wd[(e + 1) % 2], f"moe_w_down{e + 1}", 0, D)
                        j0, j1 = HALF[hf]
                        for j in range(j0, j1):
                            wg_ = wgh[u % 2][:, :, (j - j0) * 128:(j - j0 + 1) * 128]
                            wu_ = wuh[u % 2][:, :, (j - j0) * 128:(j - j0 + 1) * 128]
                            for n in range(TB // 512):
                                pg, pu = k.psum(), k.psum()
                                for ps, w_ in ((pg, wg_), (pu, wu_)):
                                    for kc in range(8):
                                        k.mm(ps, w_[:, kc, :], uT[:, kc, n * 512:(n + 1) * 512].on(*ub[4 * n:4 * n + 4]),
                                             start=kc == 0, stop=kc == 7)
                                s_ = sl[it % 2]
                                it += 1
                                k.activation(s_, pg, AF.Silu)
                                k.tt(hT[:, j, n * 512:(n + 1) * 512].on(hb[j][n]), s_, pu, ALU.mult)
                    for t in range(NTB):
                        for nn in range(2):
                            ps = k.psum()
                            for j in range(NJ):
                                k.mm(ps, hT[:, j, t * 128:(t + 1) * 128].on(hb[j][t // 4]), wde[:, j, nn * 512:(nn + 1) * 512], start=j == 0, stop=j == NJ - 1)
                            a_ = acc[:, t, nn * 512:(nn + 1) * 512].on(ab[t])
                            k.stt(a_, ps, comb[:, t, e:e + 1], a_, ALU.mult, ALU.add)
                for t in range(NTB):
                    k.dma(self.rows(hout, s, blk * NTB + t), acc[:, t, :].on(ab[t]), q=k.pool)


_CACHE = {}


def _run(inputs, nseq, ncores, stop="end"):
    key = (nseq, stop)
    if key not in _CACHE:
        p = Prog(nseq, stop)
        _CACHE[key] = (p.build(), p)
    nc = _CACHE[key][0]
    cst = make_consts(inputs)
    hw = host_weights(inputs)
    x = np.asarray(inputs["x"], dtype=np.float32)
    mem = np.asarray(inputs["mem"], dtype=np.float32)
    in_maps = []
    for c in range(ncores):
        m = {"x": np.ascontiguousarray(x[c * nseq:(c + 1) * nseq].reshape(nseq * S, D)),
             "mem": np.ascontiguousarray(mem[c * nseq:(c + 1) * nseq].reshape(nseq * NMEM, D)),
             "cst": cst}
        m.update(hw)
        in_maps.append(m)
    res = run_bass_kernel_spmd(nc, in_maps, core_ids=list(range(ncores)))
    global _LAST
    _LAST = res
    return np.concatenate([r["out"].reshape(nseq, S, D) for r in res.results], axis=0)


def kernel(**inputs):
    return _run(inputs, 4, 8)
```

```python
import contextlib
import numpy as np
import concourse.bass as bass
import concourse.mybir as mybir
from concourse.bass_utils import run_bass_kernel_spmd

F32 = mybir.dt.float32
BF16 = mybir.dt.bfloat16
AF = mybir.ActivationFunctionType
ALU = mybir.AluOpType
AX = mybir.AxisListType


class Buf:
    __slots__ = ("name", "w", "r", "excl")

    def __init__(self, name="", excl=False):
        self.name = name
        self.excl = excl
        self.w = []
        self.r = []


class V:
    __slots__ = ("ap", "bufs")

    def __init__(self, ap, bufs):
        self.ap = ap
        self.bufs = tuple(bufs)

    def __getitem__(self, idx):
        return V(self.ap[idx], self.bufs)

    def bitcast(self, dt):
        return V(self.ap.bitcast(dt), self.bufs)

    def rearrange(self, s, **kw):
        return V(self.ap.rearrange(s, **kw), self.bufs)

    def bc(self, shape):
        return V(self.ap.to_broadcast(shape), self.bufs)

    def on(self, *bufs):
        return V(self.ap, bufs)

    @property
    def shape(self):
        return self.ap.shape


class Eng:
    def __init__(self, name, h, sem, self_sync):
        self.name, self.h, self.sem, self.self_sync = name, h, sem, self_sync
        self.cnt = 0
        self.known = {}


class K:
    def __init__(self, nc, es, n_dma_sems=40):
        self.nc, self.es = nc, es
        mk = lambda n: es.enter_context(nc.semaphore(n))
        self.pe = Eng("pe", nc.tensor, mk("s_pe"), False)
        self.act = Eng("act", nc.scalar, mk("s_act"), True)
        self.dve = Eng("dve", nc.vector, mk("s_dve"), True)
        self.pool = Eng("pool", nc.gpsimd, mk("s_pool"), True)
        self.sp = Eng("sp", nc.sync, mk("s_sp"), False)
        self.engs = [self.pe, self.act, self.dve, self.pool, self.sp]
        self.dsems = [[mk(f"s_d{i}"), 0] for i in range(n_dma_sems)]
        self.dsems_sw = [[mk(f"s_w{i}"), 0] for i in range(24)]
        self.dnext = 0
        self.dnext_sw = 0
        self.ps = []
        for i in range(8):
            t = es.enter_context(nc.psum_tensor(f"psb{i}", [128, 512], F32))
            self.ps.append(V(t[:, :], [Buf(f"ps{i}", excl=True)]))
        self.psn = 0
        self.held = set()
        self.ntile = 0
        self.ninst = 0

    @contextlib.contextmanager
    def scope(self):
        with contextlib.ExitStack() as es:
            yield es
            self.barrier()

    def psum(self, hold=False):
        while (self.psn % 8) in self.held:
            self.psn += 1
        i = self.psn % 8
        self.psn += 1
        if hold:
            self.held.add(i)
        return self.ps[i]

    def release(self, p):
        self.held.discard(self.ps.index(p))

    def sb(self, shape, dt, name=None, es=None):
        self.ntile += 1
        name = name or f"t{self.ntile}"
        t = (es or self.es).enter_context(self.nc.sbuf_tensor(f"{name}_{self.ntile}", list(shape), dt))
        idx = tuple(slice(None) for _ in shape)
        return V(t[idx], [Buf(name)])

    def dram(self, name, shape, dt, kind="Internal"):
        t = self.nc.dram_tensor(name, list(shape), dt, kind=kind)
        return V(t.ap(), [Buf(name)])

    def _deps(self, e, reads, writes):
        deps = {}

        def need(tok):
            s, v = tok
            k = id(s)
            if k not in deps or deps[k][1] < v:
                deps[k] = (s, v)
        for b in reads:
            for tok in b.w:
                need(tok)
            if b.excl:
                for tok in b.r:
                    need(tok)
        for b in writes:
            for tok in b.w:
                need(tok)
            for tok in b.r:
                need(tok)
        for k, (s, v) in deps.items():
            if s is e.sem and not e.self_sync:
                continue
            if e.known.get(k, 0) >= v:
                continue
            e.h.wait_ge(s, v)
            e.known[k] = v

    def _mark(self, tok, reads, writes):
        for b in writes:
            b.w = [tok]
            b.r = []
        ws = set(id(b) for b in writes)
        for b in reads:
            if id(b) in ws:
                continue
            b.r = [x for x in b.r if x[0] is not tok[0]] + [tok]

    def op(self, e, emit, ins, outs, rd=(), wr=()):
        reads, writes = list(rd), list(wr)
        for v in ins:
            if isinstance(v, V):
                reads.extend(v.bufs)
        for v in outs:
            writes.extend(v.bufs)
        self._deps(e, reads, writes)
        ins_ = emit()
        e.cnt += 1
        ins_.then_inc(e.sem, 1)
        self._mark((e.sem, e.cnt), reads, writes)
        self.ninst += 1
        return ins_

    def dma(self, out, in_, q=None, rd=(), wr=()):
        e = q or self.sp
        reads = list(in_.bufs) + list(rd)
        writes = list(out.bufs) + list(wr)
        if e is self.pool:
            ds = self.dsems_sw[self.dnext_sw % len(self.dsems_sw)]
            self.dnext_sw += 1
        else:
            ds = self.dsems[self.dnext % len(self.dsems)]
            self.dnext += 1
        self._deps(e, reads, writes)
        if ds[1] > 0 and e.known.get(id(ds[0]), 0) < ds[1]:
            e.h.wait_ge(ds[0], ds[1])
            e.known[id(ds[0])] = ds[1]
        ins_ = e.h.dma_start(out=out.ap, in_=in_.ap)
        ds[1] += 16
        ins_.then_inc(ds[0], 16)
        self._mark((ds[0], ds[1]), reads, writes)
        self.ninst += 1

    def barrier(self):
        toks = [(x.sem, x.cnt) for x in self.engs if x.cnt > 0] + [(d[0], d[1]) for d in self.dsems + self.dsems_sw if d[1] > 0]
        for e in self.engs:
            for s, v in toks:
                if s is e.sem:
                    continue
                if e.known.get(id(s), 0) >= v:
                    continue
                e.h.wait_ge(s, v)
                e.known[id(s)] = v

    @staticmethod
    def _a(x):
        return x.ap if isinstance(x, V) else x

    def mm(self, out, lhsT, rhs, start=True, stop=True):
        return self.op(self.pe, lambda: self.nc.tensor.matmul(out.ap, lhsT.ap, rhs.ap, start=start, stop=stop),
                       [lhsT, rhs], [out])

    def tr(self, out, in_, ident):
        return self.op(self.pe, lambda: self.nc.tensor.transpose(out.ap, in_.ap, ident.ap), [in_, ident], [out])

    def activation(self, out, in_, func, bias=None, scale=None, accum_out=None, e=None):
        e = e or self.act
        kw = {}
        if bias is not None:
            kw["bias"] = self._a(bias)
        if scale is not None:
            kw["scale"] = self._a(scale)
        outs = [out]
        if accum_out is not None:
            kw["accum_out"] = accum_out.ap
            outs.append(accum_out)
        return self.op(e, lambda: e.h.activation(out=out.ap, in_=in_.ap, func=func, **kw),
                       [in_, bias, scale], outs)

    def tt(self, out, in0, in1, op, e=None):
        e = e or self.dve
        return self.op(e, lambda: e.h.tensor_tensor(out=out.ap, in0=in0.ap, in1=in1.ap, op=op), [in0, in1], [out])

    def ts(self, out, in0, s1, op0, s2=None, op1=None, e=None, accum_out=None):
        e = e or self.dve
        kw = {}
        if op1 is not None:
            kw["op1"] = op1
        outs = [out]
        if accum_out is not None:
            kw["accum_out"] = accum_out.ap
            outs.append(accum_out)
        return self.op(e, lambda: e.h.tensor_scalar(out=out.ap, in0=in0.ap, scalar1=self._a(s1), scalar2=self._a(s2),
                                                    op0=op0, **kw), [in0, s1, s2], outs)

    def stt(self, out, in0, scalar, in1, op0, op1, e=None):
        e = e or self.dve
        return self.op(e, lambda: e.h.scalar_tensor_tensor(out=out.ap, in0=in0.ap, scalar=self._a(scalar), in1=in1.ap,
                                                           op0=op0, op1=op1), [in0, scalar, in1], [out])

    def copy(self, out, in_, e=None):
        e = e or self.dve
        if e is self.act:
            return self.op(e, lambda: e.h.copy(out=out.ap, in_=in_.ap), [in_], [out])
        return self.op(e, lambda: e.h.tensor_copy(out=out.ap, in_=in_.ap), [in_], [out])

    def memset(self, out, val, e=None):
        e = e or self.dve
        return self.op(e, lambda: e.h.memset(out.ap, val), [], [out])

    def recip(self, out, in_, e=None):
        e = e or self.dve
        return self.op(e, lambda: e.h.reciprocal(out=out.ap, in_=in_.ap), [in_], [out])

    def recip_fast(self, out, in_):
        e = self.dve
        return self.op(e, lambda: e.h.reciprocal_approx_fast(out=out.ap, in_=in_.ap), [in_], [out])

    def reduce(self, out, in_, op, axis=AX.X, e=None):
        e = e or self.dve
        return self.op(e, lambda: e.h.tensor_reduce(out=out.ap, in_=in_.ap, axis=axis, op=op), [in_], [out])

    def max8(self, out, in_):
        e = self.dve
        return self.op(e, lambda: e.h.max(out=out.ap, in_=in_.ap), [in_], [out])


S = 2048
D = 1024
NMEM = 256
EPS = 1e-6
DFF = 2816
DFE = 1408
NEXP = 8

CST = {}
_off = 0
def _reg(name, w):
    global _off
    CST[name] = (_off, w)
    _off += w
for _n in ("norm_mix", "norm_xattn", "norm_mem", "norm_ffn"):
    for _l in range(2):
        _reg(f"{_n}{_l}", 8)
_reg("ev_conv", 12); _reg("ev_qn", 1); _reg("ev_kn", 1)
for _l in range(2):
    _reg(f"xa_qn{_l}", 1); _reg(f"xa_kn{_l}", 1)
_reg("od_conv", 96); _reg("od_alog", 8); _reg("od_dtb", 8); _reg("od_on", 1)
_reg("ident", 128); _reg("bones64", 128); _reg("ones", 128); _reg("mask", 256)
_reg("tri_incl", 128)
_reg("blk64", 128)
_reg("strict_ji", 128)
_reg("causal_ji", 128)
_reg("strict_ij", 128)
_reg("gtk", 128)
NCST = _off


def make_consts(inp):
    c = np.zeros((128, NCST), np.float32)
    def put(name, arr):
        o, w = CST[name]
        assert arr.shape == (128, w), (name, arr.shape, w)
        c[:, o:o + w] = arr
    for n in ("norm_mix", "norm_xattn", "norm_mem", "norm_ffn"):
        for l in range(2):
            put(f"{n}{l}", np.asarray(inp[n][l]).reshape(8, 128).T)
    put("ev_conv", np.asarray(inp["ev_conv"][0]).reshape(3, 4, 128).transpose(2, 1, 0).reshape(128, 12))
    put("ev_qn", np.tile(np.asarray(inp["ev_q_norm"][0]), 2).reshape(128, 1))
    put("ev_kn", np.tile(np.asarray(inp["ev_k_norm"][0]), 2).reshape(128, 1))
    for l in range(2):
        put(f"xa_qn{l}", np.tile(np.asarray(inp["xa_q_norm"][l]), 2).reshape(128, 1))
        put(f"xa_kn{l}", np.tile(np.asarray(inp["xa_k_norm"][l]), 2).reshape(128, 1))
    put("od_conv", np.asarray(inp["od_conv"][0]).reshape(4, 24, 128).transpose(2, 1, 0).reshape(128, 96))
    put("od_alog", np.broadcast_to(np.asarray(inp["od_a_log"][0]).reshape(1, 8), (128, 8)))
    put("od_dtb", np.broadcast_to(np.asarray(inp["od_dt_bias"][0]).reshape(1, 8), (128, 8)))
    put("od_on", np.asarray(inp["od_o_norm"][0]).reshape(128, 1))
    i = np.arange(128)
    put("ident", np.eye(128, dtype=np.float32))
    put("bones64", (i[:, None] // 64 == i[None, :] // 64).astype(np.float32))
    put("ones", np.ones((128, 128), np.float32))
    msame = (i[:, None] <= i[None, :]).astype(np.float32)
    mprev = (i[:, None] >= i[None, :]).astype(np.float32)
    put("mask", np.concatenate([msame, mprev], 1))
    same = (i[:, None] // 64 == i[None, :] // 64)
    put("tri_incl", ((i[:, None] <= i[None, :]) & same).astype(np.float32))
    put("blk64", same.astype(np.float32))
    put("strict_ji", ((i[:, None] < i[None, :]) & same).astype(np.float32))
    put("causal_ji", ((i[:, None] <= i[None, :]) & same).astype(np.float32))
    put("strict_ij", ((i[None, :] < i[:, None]) & same).astype(np.float32))
    put("gtk", (i[:, None] > i[None, :]).astype(np.float32))
    return c


WEIGHTS = [
    ("ev_w_in", 1024, 3072, "norm_mix0"), ("ev_w_out", 1024, 1024, None),
    ("od_w_in", 1024, 4112, "norm_mix1"), ("od_w_out", 1024, 1024, None),
    ("xa_w_q0", 1024, 256, "norm_xattn0"), ("xa_w_kv0", 1024, 512, "norm_mem0"), ("xa_w_o0", 256, 1024, None),
    ("xa_w_q1", 1024, 256, "norm_xattn1"), ("xa_w_kv1", 1024, 512, "norm_mem1"), ("xa_w_o1", 256, 1024, None),
    ("ff_w_gu", 1024, 5632, "norm_ffn0"), ("ff_w_down", 2816, 1024, None),
    ("moe_router", 1024, 8, "norm_ffn1"),
] + [(f"moe_w_gu{e}", 1024, 2816, "norm_ffn1") for e in range(8)] + [(f"moe_w_down{e}", 1408, 1024, None) for e in range(8)]


def host_weights(inp):
    w = {}
    w["ev_w_in"] = inp["ev_w_in"][0]; w["ev_w_out"] = inp["ev_w_out"][0]
    w["od_w_in"] = inp["od_w_in"][0]; w["od_w_out"] = inp["od_w_out"][0]
    for l in range(2):
        w[f"xa_w_q{l}"] = inp["xa_w_q"][l]; w[f"xa_w_kv{l}"] = inp["xa_w_kv"][l]; w[f"xa_w_o{l}"] = inp["xa_w_o"][l]
    w["ff_w_gu"] = inp["ff_w_gu"][0]; w["ff_w_down"] = inp["ff_w_down"][0]
    w["moe_router"] = inp["moe_router"][0]
    for e in range(8):
        w[f"moe_w_gu{e}"] = inp["moe_w_gu"][0, e]; w[f"moe_w_down{e}"] = inp["moe_w_down"][0, e]
    return {k: np.ascontiguousarray(v, dtype=np.float32) for k, v in w.items()}


def ssl(start, n, step):
    return slice(start, start + (n - 1) * step + 1, step)


class Prog:
    def __init__(self, nseq, stop="end"):
        self.nseq, self.stop = nseq, stop
        self.dbgs = []
        self.nc = nc = bass.Bass("TRN2", target_bir_lowering=False)
        self.es = contextlib.ExitStack()
        NT = nseq * S
        ext = lambda n, shp, kind: V(nc.dram_tensor(n, list(shp), F32, kind=kind).ap(), [Buf(n)])
        self.x = ext("x", [NT, D], "ExternalInput")
        self.mem = ext("mem", [nseq * NMEM, D], "ExternalInput")
        self.cst_d = ext("cst", [128, NCST], "ExternalInput")
        self.w32 = {n: ext(n, [r, c], "ExternalInput") for n, r, c, g in WEIGHTS}
        self.out = ext("out", [NT, D], "ExternalOutput")

    def build(self):
        with self.es:
            self.k = K(self.nc, self.es)
            self._build()
            self.k.barrier()
        return self.nc

    def _build(self):
        k, nc, nseq = self.k, self.nc, self.nseq
        NT = nseq * S
        idr = k.dram
        self.wb = {n: idr("wb_" + n, [r, c], BF16) for n, r, c, g in WEIGHTS}
        self.hA = idr("hA", [NT, D], F32)
        self.hB = idr("hB", [NT, D], F32)
        self.att = idr("att", [3, NT, 520], F32)
        self.ogd = idr("ogd", [nseq, 1024, S], BF16)
        self.hbuf = {id(t): [Buf() for _ in range(NT // 128)] for t in (self.x, self.hA, self.hB, self.out)}
        self.cst = k.sb([128, NCST], F32, "cst")
        k.dma(self.cst, self.cst_d)
        self.cb = {}
        for n in ("ident", "bones64", "ones", "mask"):
            o, w = CST[n]
            t = k.sb([128, w], BF16, "c_" + n)
            k.copy(t, self.cst[:, o:o + w])
            self.cb[n] = t
        self.dnc = {}
        for n in ("tri_incl", "blk64", "gtk"):
            o, w = CST[n]
            t_ = k.sb([128, w], BF16, "c_" + n)
            k.copy(t_, self.cst[:, o:o + w])
            self.dnc[n] = t_
        self.eps = k.sb([128, 1], F32, "eps")
        k.memset(self.eps, EPS)
        self.gq8 = k.sb([128, 4], F32, "gq8")
        k.activation(self.gq8[:, 0:1], self.c("ev_qn"), AF.Copy, scale=0.125)
        k.activation(self.gq8[:, 1:2], self.c("xa_qn0"), AF.Copy, scale=0.125)
        k.activation(self.gq8[:, 2:3], self.c("xa_qn1"), AF.Copy, scale=0.125)

        if self.stop == "scantest":
            NTT = 4
            tq = V(nc.dram_tensor("tq", [128, 24, NTT * 128], F32, kind="ExternalInput").ap(), [Buf()])
            tbg = V(nc.dram_tensor("tbg", [128, NTT, 16], F32, kind="ExternalInput").ap(), [Buf()])
            tog = V(nc.dram_tensor("tog", [128, NTT, 8, 128], F32, kind="ExternalOutput").ap(), [Buf()])
            with k.scope() as es:
                q32 = k.sb([128, 24, NTT * 128], F32, "q32", es)
                qkvT = k.sb([128, 24, NTT * 128], BF16, "qkvT", es)
                bg = k.sb([128, NTT, 16], F32, "bg", es)
                ogf = k.sb([128, NTT, 8, 128], F32, "ogf", es)
                k.dma(q32, tq)
                k.dma(bg, tbg)
                k.copy(qkvT, q32)
                self.dn_scan(qkvT, bg, NTT, lambda t, og: k.copy(ogf[:, t, :, :], og, e=k.pool))
                k.dma(tog, ogf)
            return
        if self.layer0() == "stop":
            return
        for s in range(nseq):
            self.deltanet(s, self.hB, self.hA)
            k.barrier()
        if self.stop.startswith("mix1"):
            return self.copy_out(self.hA)
        for s in range(nseq):
            self.xattn(s, 1, self.hA, self.hB)
            k.barrier()
        if self.stop == "xa1":
            return self.copy_out(self.hB)
        for s in range(nseq):
            self.moe(s, self.hB, self.out)
            k.barrier()

    def layer0(self):
        k, nseq = self.k, self.nseq
        with k.scope() as esbg:
            L0W = ("ev_w_in", "ev_w_out")
            self.bg32 = [k.sb([128, 1024], F32, "bg32", esbg) for _ in range(3)]
            self.bgb = [k.sb([128, 1024], BF16, "bgb", esbg) for _ in range(3)]
            self.phase_convert(L0W)
            k.barrier()
            if self.stop == "convert":
                return "stop"
            order = ["xa_w_q0", "xa_w_kv0", "xa_w_o0", "ff_w_gu", "ff_w_down"]
            order += [n for n, r, c, g in WEIGHTS if n not in L0W and n not in order]
            self.bg_done = set()
            self.bg = self.bg_convert_gen(order)
            for s in range(nseq):
                self.l0_mixer(s)
                k.barrier()
            if self.stop.startswith("mix0"):
                self.copy_out(self.hB)
                return "stop"
            self.drain_until(("xa_w_q0", "xa_w_kv0", "xa_w_o0"))
            for s in range(nseq):
                self.xattn(s, 0, self.hB, self.hA)
                k.barrier()
            if self.stop == "xa0":
                self.copy_out(self.hA)
                return "stop"
            last = self.stop == "ffn0"
            self.drain_until(("ff_w_gu", "ff_w_down"))
            for s in range(nseq):
                self.ffn(s, self.hA, self.out if last else self.hB)
                k.barrier()
            if last:
                return "stop"
            for _ in self.bg:
                pass
            k.barrier()
        return "ok"

    def dbg(self, name, v, es):
        if not (self.stop.endswith("dbg") or self.stop == "scantest"):
            return
        import os
        sel = os.environ.get("DBGSEL")
        if sel is not None and name not in sel.split(","):
            return
        k = self.k
        shp = list(v.shape)
        t32 = k.sb(shp, F32, "dbg", es)
        k.copy(t32, v)
        d = V(self.nc.dram_tensor("dbg_" + name, shp, F32, kind="ExternalOutput").ap(), [Buf()])
        k.dma(d, t32)
        self.dbgs.append("dbg_" + name)

    def c(self, name):
        o, w = CST[name]
        return self.cst[:, o:o + w]

    def rows(self, t, s, i, n=1):
        g = s * (S // 128) + i
        return t[g * 128:(g + n) * 128, :].on(*self.hbuf[id(t)][g:g + n])

    def copy_out(self, src):
        k = self.k
        with k.scope() as es:
            bufs = [k.sb([128, D], F32, "co", es) for _ in range(3)]
            for s in range(self.nseq):
                for i in range(S // 128):
                    b = bufs[i % 3]
                    k.dma(b, self.rows(src, s, i))
                    k.dma(self.rows(self.out, s, i), b, q=k.pool)
        k.barrier()

    def phase_convert(self, names):
        k = self.k
        CBLK = 2048
        with k.scope() as es:
            st32 = [k.sb([128, CBLK], F32, "st32", es) for _ in range(3)]
            stb = [k.sb([128, CBLK], BF16, "stb", es) for _ in range(3)]
            it = 0
            engs = [k.dve, k.act, k.dve]
            for n, r, cdim, g in WEIGHTS:
                if n not in names:
                    continue
                for kc in range(r // 128):
                    for c0 in range(0, cdim, CBLK):
                        cbk = min(CBLK, cdim - c0)
                        a, b, e = st32[it % 3], stb[it % 3], engs[it % 3]
                        it += 1
                        k.dma(a[:, :cbk], self.w32[n][kc * 128:(kc + 1) * 128, c0:c0 + cbk])
                        if g is None:
                            k.copy(b[:, :cbk], a[:, :cbk], e=e)
                        else:
                            gcol = self.c(g)[:, kc:kc + 1]
                            if e is k.act:
                                k.activation(b[:, :cbk], a[:, :cbk], AF.Identity, scale=gcol)
                            else:
                                k.ts(b[:, :cbk], a[:, :cbk], gcol, ALU.mult, e=e)
                        k.dma(self.wb[n][kc * 128:(kc + 1) * 128, c0:c0 + cbk], b[:, :cbk], q=k.act)

    def bg_convert_gen(self, names):
        k = self.k
        CB = 1024
        NB = len(self.bg32)
        blocks = []
        wd_ = {n: (r, cdim, g) for n, r, cdim, g in WEIGHTS}
        for n in names:
            r, cdim, g = wd_[n]
            for kc in range(r // 128):
                for c0 in range(0, cdim, CB):
                    blocks.append((n, kc, c0, min(CB, cdim - c0), g))
        last_of = {blk[0]: i for i, blk in enumerate(blocks)}

        def finish(i):
            n, kc, c0, cbk, g = blocks[i]
            if last_of[n] == i:
                self.bg_done.add(n)
            a, b = self.bg32[i % NB], self.bgb[i % NB]
            if g is None:
                k.copy(b[:, :cbk], a[:, :cbk], e=k.act)
            else:
                k.activation(b[:, :cbk], a[:, :cbk], AF.Identity, scale=self.c(g)[:, kc:kc + 1])
            k.dma(self.wb[n][kc * 128:(kc + 1) * 128, c0:c0 + cbk], b[:, :cbk], q=k.act)
        for i, (n, kc, c0, cbk, g) in enumerate(blocks):
            k.dma(self.bg32[i % NB][:, :cbk], self.w32[n][kc * 128:(kc + 1) * 128, c0:c0 + cbk])
            if i > 1:
                finish(i - 2)
            yield
        for i in range(max(0, len(blocks) - 2), len(blocks)):
            finish(i)

    def drain_until(self, names):
        while not set(names) <= self.bg_done:
            if next(self.bg, "done") == "done":
                break
        self.k.barrier()

    def pump(self, n=1):
        for _ in range(n):
            if next(self.bg, "done") == "done":
                break

    def norm_T(self, es, src, s, uT, ub, ntiles=S // 128, tile0=0, rows_fn=None):
        k = self.k
        if isinstance(es, dict):
            nb = es
        else:
            nb = self.norm_bufs(es, ntiles)
        hts, xns, junk, st = nb["hts"], nb["xns"], nb["junk"], nb["st"]
        for t in range(ntiles):
            ht, xn = hts[t % len(hts)], xns[t % 2]
            k.dma(ht, rows_fn(t) if rows_fn else self.rows(src, s, tile0 + t))
            ss, rs, rstd = st[:, 3 * t:3 * t + 1], st[:, 3 * t + 1:3 * t + 2], st[:, 3 * t + 2:3 * t + 3]
            k.activation(junk, ht, AF.Square)
            k.reduce(ss, junk, ALU.add)
            k.activation(rs, ss, AF.Sqrt, scale=1.0 / D, bias=self.eps)
            k.recip(rstd, rs)
            k.ts(xn, ht, rstd, ALU.mult)
            ps = k.psum()
            psb = ps.bitcast(BF16)
            for c in range(8):
                k.tr(psb[:, c * 128:(c + 1) * 128], xn[:, c * 128:(c + 1) * 128], self.cb["ident"])
            k.copy(uT[:, :, t * 128:(t + 1) * 128].on(ub[t]), psb.rearrange("p (c t) -> p c t", c=8),
                   e=k.act if t % 2 else k.dve)

    def norm_bufs(self, es, ntiles=S // 128, nh=3):
        k = self.k
        return {"hts": [k.sb([128, D], F32, "nt_h", es) for _ in range(nh)],
                "xns": [k.sb([128, D], BF16, "nt_x", es) for _ in range(2)],
                "junk": k.sb([128, D], F32, "nt_j", es),
                "st": k.sb([128, 3 * ntiles], F32, "nt_s", es)}

    def wload(self, dst, name, c0, cw, kc0=0, nkc=None):
        k = self.k
        w = self.wb[name]
        nkc = nkc if nkc is not None else w.shape[0] // 128
        src = w[kc0 * 128:(kc0 + nkc) * 128, c0:c0 + cw].rearrange("(kc p) c -> p kc c", p=128)
        k.dma(dst, src)

    def headnorm(self, ps, out, gcol, ones_name, inv_dim, pool, n):
        k = self.k
        sq, r = pool["sq"][n % 2], pool["r"][n % 2]
        w = ps.shape[-1]
        k.activation(sq[:, :w], ps, AF.Square)
        ps2 = k.psum()
        k.mm(ps2[:, :w], self.cb[ones_name], sq[:, :w])
        k.activation(r[:, :w], ps2[:, :w], AF.Ln, scale=inv_dim, bias=self.eps)
        k.activation(r[:, :w], r[:, :w], AF.Exp, scale=-0.5)
        k.stt(out, ps, gcol, r[:, :w], ALU.mult, ALU.mult)

    def l0_mixer(self, s):
        k = self.k
        NTL = S // 128
        with k.scope() as es0:
            aT = k.sb([128, 4, S], BF16, "aT", es0)
            with k.scope() as es1:
                qT = k.sb([128, 4, S], BF16, "qT", es1)
                kT = k.sb([128, 4, S], BF16, "kT", es1)
                uT = k.sb([128, 8, S], BF16, "uT", es1)
                ub = [Buf() for _ in range(NTL)]
                wbl = [k.sb([128, 8, 512], BF16, "wbl", es1) for _ in range(3)]
                self.norm_T(self.norm_bufs(es1, nh=2), self.x, s, uT, ub)
                self.dbg("uT", uT[:, :, 0:256], es1)
                for i in range(3):
                    self.wload(wbl[i], "ev_w_in", i * 512, 512)
                with k.scope() as es:
                    ys = [k.sb([128, S + 2], F32, "y", es) for _ in range(2)]
                    gbs = [k.sb([128, S], BF16, "gbs", es) for _ in range(2)]
                    tmp = [k.sb([128, 512], F32, "tgc", es) for _ in range(2)]
                    acc = [k.sb([128, S], F32, "acc", es) for _ in range(2)]
                    for y in ys:
                        k.memset(y[:, 0:2], 0.0)
                    cw = self.c("ev_conv")
                    for c in range(4):
                        y, gb = ys[c % 2], gbs[c % 2]
                        for n in range(4):
                            pss = [k.psum() for _ in range(3)]
                            for j in range(3):
                                for kc in range(8):
                                    k.mm(pss[j], wbl[j][:, kc, c * 128:(c + 1) * 128],
                                         uT[:, kc, n * 512:(n + 1) * 512].on(*ub[4 * n:4 * n + 4]), start=kc == 0, stop=kc == 7)
                            t_ = tmp[n % 2]
                            k.activation(t_, pss[1], AF.Copy)
                            k.tt(y[:, 2 + n * 512:2 + (n + 1) * 512], t_, pss[2], ALU.mult)
                            k.activation(gb[:, n * 512:(n + 1) * 512], pss[0], AF.Copy)
                            self.pump()
                        a_ = acc[c % 2]
                        k.ts(a_, y[:, 2:S + 2], cw[:, c * 3 + 2:c * 3 + 3], ALU.mult)
                        k.stt(a_, y[:, 1:S + 1], cw[:, c * 3 + 1:c * 3 + 2], a_, ALU.mult, ALU.add)
                        k.stt(a_, y[:, 0:S], cw[:, c * 3 + 0:c * 3 + 1], a_, ALU.mult, ALU.add)
                        k.tt(aT[:, c, :], a_, gb, ALU.mult, e=k.pool)
                for i in range(3):
                    self.wload(wbl[i], "ev_w_in", 1536 + i * 512, 512)
                with k.scope() as es:
                    pool = {"sq": [k.sb([128, 512], BF16, "sq", es) for _ in range(2)],
                            "r": [k.sb([128, 512], F32, "r", es) for _ in range(2)]}
                    it = 0
                    for j, (XT, g) in enumerate(((qT, self.gq8[:, 0:1]), (kT, self.c("ev_kn")))):
                        for c in range(4):
                            for n in range(4):
                                ps = k.psum()
                                for kc in range(8):
                                    k.mm(ps, wbl[j][:, kc, c * 128:(c + 1) * 128],
                                         uT[:, kc, n * 512:(n + 1) * 512].on(*ub[4 * n:4 * n + 4]), start=kc == 0, stop=kc == 7)
                                self.headnorm(ps, XT[:, c, n * 512:(n + 1) * 512], g, "bones64", 1.0 / 64, pool, it)
                                it += 1
                                self.pump()
                self.dbg("aT", aT[:, :, 0:256], es1)
                self.dbg("qT", qT[:, :, 0:256], es1)
                self.dbg("kT", kT[:, :, 0:256], es1)
                with k.scope() as es:
                    vA = k.sb([128, 16, 8, 65], BF16, "vA", es)
                    k.memset(vA[:, :, :, 64:65], 1.0, e=k.pool)
                    P = [[k.sb([128, 2, 256], BF16, "P", es) for _ in range(3)] for _ in range(4)]
                    osb = [k.sb([128, 8, 65], F32, "osb", es) for _ in range(2)]
                    m512 = k.sb([128, 2, 256], BF16, "m512", es)
                    k.copy(m512[:, 0, :], self.cb["mask"])
                    k.copy(m512[:, 1, :], self.cb["mask"])
                    nq_it = 0
                    for pi, d in enumerate((1, 4, 16)):
                        nb = 16 // d
                        att = self.att[pi]
                        vb = [Buf() for _ in range(16)]
                        for r in range(d):
                            for lb in range(nb):
                                blk = r * nb + lb
                                st = lb * 128 * d + r
                                ps = k.psum()
                                tl = ub[lb * d:(lb + 1) * d]
                                for kc in range(8):
                                    k.mm(ps, uT[:, kc, ssl(st, 128, d)].on(*tl), wbl[2][:, kc, :], start=kc == 0, stop=kc == 7)
                                k.copy(vA[:, blk, :, 0:64].on(vb[blk]), ps.rearrange("p (h e) -> p h e", h=8),
                                       e=k.act if blk % 2 else k.dve)
                        if pi == 0:
                            self.dbg("vA", vA[:, 0:2, :, :].rearrange("p a h e -> p (a h e)"), es)
                        for r in range(d):
                            def scores(kb):
                                nq = 256 if kb + 1 < nb else 128
                                ks = kb * 128 * d + r
                                for g in range(4):
                                    e_, hq = g // 2, g % 2
                                    p0 = e_ * 64
                                    ps = k.psum()
                                    for i in range(2):
                                        hp = 2 * hq + i
                                        k.mm(ps[:, i * 256:i * 256 + nq], kT[p0:p0 + 64, hp, ssl(ks, 128, d)],
                                             qT[p0:p0 + 64, hp, ssl(ks, nq, d)])
                                    Pv = P[g][kb % 3][:, :, :nq]
                                    pv = ps.rearrange("p (e q) -> p e q", e=2)[:, :, :nq]
                                    k.activation(Pv, pv, AF.Exp)
                                    k.tt(Pv, Pv, m512[:, :, :nq], ALU.mult, e=k.pool if g % 2 else k.dve)
                            scores(0)
                            for kb in range(nb):
                                if kb + 1 < nb:
                                    scores(kb + 1)
                                pso = [k.psum(), k.psum()]
                                for h in range(8):
                                    hp, e_ = h // 2, h % 2
                                    g, i = e_ * 2 + hp // 2, hp % 2
                                    reg = pso[h // 4][:, (h % 4) * 65:(h % 4) * 65 + 65]
                                    if kb > 0:
                                        b0 = r * nb + kb - 1
                                        k.mm(reg, P[g][(kb - 1) % 3][:, i, 128:256], vA[:, b0, h, :].on(vb[b0], *vA.bufs), start=True, stop=False)
                                    b1 = r * nb + kb
                                    k.mm(reg, P[g][kb % 3][:, i, 0:128], vA[:, b1, h, :].on(vb[b1], *vA.bufs), start=kb == 0, stop=True)
                                ob = osb[nq_it % 2]
                                for half in range(2):
                                    k.copy(ob[:, half * 4:(half + 1) * 4, :],
                                           pso[half][:, 0:260].rearrange("p (h e) -> p h e", h=4),
                                           e=k.act if nq_it % 2 else k.dve)
                                nq_it += 1
                                base = s * S + kb * 128 * d + r
                                dst = att[ssl(base, 128, d), :]
                                k.dma(dst.on(Buf()), ob.rearrange("p h e -> p (h e)"), q=k.pool)
                                self.pump()
            k.barrier()
            with k.scope() as es:
                wout = k.sb([128, 8, D], BF16, "wout", es)
                self.wload(wout, "ev_w_out", 0, D)
                NBF = 3
                a3 = [k.sb([128, 3, 520], F32, "a3", es) for _ in range(NBF)]
                sm = [k.sb([128, 520], F32, "sm", es) for _ in range(NBF)]
                rd = [k.sb([128, 8], F32, "rd", es) for _ in range(NBF)]
                obf = [k.sb([128, 512], BF16, "obf", es) for _ in range(NBF)]
                oTt = [k.sb([128, 4, 128], BF16, "oTt", es) for _ in range(NBF)]
                hts = [k.sb([128, D], F32, "hres", es) for _ in range(NBF)]
                hos = [k.sb([128, D], F32, "hout", es) for _ in range(NBF)]

                def tile_gen(t):
                    g = s * NTL + t
                    i = t % NBF
                    a, sm_, rd_, ob_, ht, ho, oT_ = a3[i], sm[i], rd[i], obf[i], hts[i], hos[i], oTt[i]
                    src = self.att[:, g * 128:(g + 1) * 128, :].rearrange("g t c -> t g c")
                    k.dma(a, src.on(Buf()))
                    k.dma(ht, self.rows(self.x, s, t))
                    k.tt(sm_, a[:, 0, :], a[:, 1, :], ALU.add, e=k.pool)
                    k.tt(sm_, sm_, a[:, 2, :], ALU.add, e=k.pool)
                    s3 = sm_.rearrange("p (h e) -> p h e", h=8)
                    k.recip(rd_, s3[:, :, 64])
                    k.tt(ob_.rearrange("p (h e) -> p h e", h=8), s3[:, :, 0:64],
                         rd_.rearrange("p (h o) -> p h o", o=1).bc([128, 8, 64]), ALU.mult)
                    yield
                    ps = k.psum()
                    psb = ps.bitcast(BF16)
                    for c in range(4):
                        k.tr(psb[:, c * 128:(c + 1) * 128], ob_[:, c * 128:(c + 1) * 128], self.cb["ident"])
                    k.copy(oT_, psb[:, 0:512].rearrange("p (c t) -> p c t", c=4), e=k.act)
                    yield
                    for n in range(2):
                        ps = k.psum()
                        for c in range(8):
                            lhs = aT[:, c, t * 128:(t + 1) * 128] if c < 4 else oT_[:, c - 4, :]
                            k.mm(ps, lhs, wout[:, c, n * 512:(n + 1) * 512], start=c == 0, stop=c == 7)
                        k.tt(ho[:, n * 512:(n + 1) * 512], ps, ht[:, n * 512:(n + 1) * 512], ALU.add)
                    k.dma(self.rows(self.hB, s, t), ho, q=k.pool)
                    self.pump()

                active = []
                for t in range(NTL + 2):
                    for g_ in list(active):
                        if next(g_, "done") == "done":
                            active.remove(g_)
                    if t < NTL:
                        g_ = tile_gen(t)
                        next(g_)
                        active.append(g_)
                for g_ in active:
                    for _ in g_:
                        pass

    def xattn(self, s, layer, hin, hout):
        k = self.k
        NTL = S // 128
        wq, wkv, wo = f"xa_w_q{layer}", f"xa_w_kv{layer}", f"xa_w_o{layer}"
        with k.scope() as es0:
            uT = k.sb([128, 8, S], BF16, "uT", es0)
            ub = [Buf() for _ in range(NTL)]
            mT = k.sb([128, 8, NMEM], BF16, "mT", es0)
            mb = [Buf() for _ in range(2)]
            qT = k.sb([128, 2, S], BF16, "xqT", es0)
            kT = k.sb([128, 2, NMEM], BF16, "xkT", es0)
            vA = k.sb([128, 2, 4, 65], BF16, "xvA", es0)
            k.memset(vA[:, :, :, 64:65], 1.0, e=k.pool)
            wqs = k.sb([128, 8, 256], BF16, "wqs", es0)
            wkvs = k.sb([128, 8, 512], BF16, "wkvs", es0)
            wos = k.sb([128, 2, D], BF16, "wos", es0)
            self.wload(wqs, wq, 0, 256)
            self.wload(wkvs, wkv, 0, 512)
            self.wload(wos, wo, 0, D)
            nbx = self.norm_bufs(es0)
            self.norm_T(nbx, None, s, mT, mb, ntiles=2,
                        rows_fn=lambda t: self.mem[(s * 2 + t) * 128:(s * 2 + t + 1) * 128, :])
            self.norm_T(nbx, hin, s, uT, ub)
            with k.scope() as es:
                pool = {"sq": [k.sb([128, 512], BF16, "sq", es) for _ in range(2)],
                        "r": [k.sb([128, 512], F32, "r", es) for _ in range(2)]}
                it = 0
                for c in range(2):
                    ps = k.psum()
                    for kc in range(8):
                        k.mm(ps[:, :NMEM], wkvs[:, kc, c * 128:(c + 1) * 128], mT[:, kc, :].on(*mb), start=kc == 0, stop=kc == 7)
                    self.headnorm(ps[:, :NMEM], kT[:, c, :], self.c(f"xa_kn{layer}"), "bones64", 1.0 / 64, pool, it)
                    it += 1
                for mblk in range(2):
                    ps = k.psum()
                    for kc in range(8):
                        k.mm(ps[:, :256], mT[:, kc, mblk * 128:(mblk + 1) * 128].on(mb[mblk]), wkvs[:, kc, 256:512], start=kc == 0, stop=kc == 7)
                    k.copy(vA[:, mblk, :, 0:64], ps[:, :256].rearrange("p (h e) -> p h e", h=4))
                for c in range(2):
                    for n in range(4):
                        ps = k.psum()
                        for kc in range(8):
                            k.mm(ps, wqs[:, kc, c * 128:(c + 1) * 128], uT[:, kc, n * 512:(n + 1) * 512].on(*ub[4 * n:4 * n + 4]),
                                 start=kc == 0, stop=kc == 7)
                        self.headnorm(ps, qT[:, c, n * 512:(n + 1) * 512], self.gq8[:, 1 + layer:2 + layer], "bones64", 1.0 / 64, pool, it)
                        it += 1
            with k.scope() as es:
                Pt = [[k.sb([128, 2, 512], BF16, "xP", es) for _ in range(2)] for _ in range(4)]
                NBF = 3
                osb = [k.sb([128, 4, 65], F32, "xo", es) for _ in range(NBF)]
                rd = [k.sb([128, 4], F32, "xrd", es) for _ in range(NBF)]
                obf = [k.sb([128, 256], BF16, "xob", es) for _ in range(NBF)]
                oTt = [k.sb([128, 2, 128], BF16, "xoT", es) for _ in range(NBF)]
                hts = [k.sb([128, D], F32, "hres", es) for _ in range(NBF)]
                hos = [k.sb([128, D], F32, "hout", es) for _ in range(NBF)]

                def scores(n):
                    for h in range(4):
                        c, p0 = h // 2, (h % 2) * 64
                        for mblk in range(2):
                            ps = k.psum()
                            k.mm(ps, kT[p0:p0 + 64, c, mblk * 128:(mblk + 1) * 128], qT[p0:p0 + 64, c, n * 512:(n + 1) * 512])
                            k.activation(Pt[h][n % 2][:, mblk, :], ps, AF.Exp)

                def tile_gen(t):
                    n, tt_ = t // 4, t % 4
                    i = t % NBF
                    ht, ho = hts[i], hos[i]
                    k.dma(ht, self.rows(hin, s, t))
                    pso = k.psum()
                    for h in range(4):
                        reg = pso[:, h * 65:(h + 1) * 65]
                        for mblk in range(2):
                            k.mm(reg, Pt[h][n % 2][:, mblk, tt_ * 128:(tt_ + 1) * 128], vA[:, mblk, h, :], start=mblk == 0, stop=mblk == 1)
                    o_ = osb[i]
                    k.copy(o_, pso[:, 0:260].rearrange("p (h e) -> p h e", h=4), e=k.act)
                    k.recip(rd[i], o_[:, :, 64])
                    k.tt(obf[i].rearrange("p (h e) -> p h e", h=4), o_[:, :, 0:64],
                         rd[i].rearrange("p (h o) -> p h o", o=1).bc([128, 4, 64]), ALU.mult)
                    yield
                    ps = k.psum()
                    psb = ps.bitcast(BF16)
                    for c in range(2):
                        k.tr(psb[:, c * 128:(c + 1) * 128], obf[i][:, c * 128:(c + 1) * 128], self.cb["ident"])
                    k.copy(oTt[i], psb[:, 0:256].rearrange("p (c t) -> p c t", c=2), e=k.act)
                    yield
                    for nn in range(2):
                        ps = k.psum()
                        for c in range(2):
                            k.mm(ps, oTt[i][:, c, :], wos[:, c, nn * 512:(nn + 1) * 512], start=c == 0, stop=c == 1)
                        k.tt(ho[:, nn * 512:(nn + 1) * 512], ps, ht[:, nn * 512:(nn + 1) * 512], ALU.add)
                    k.dma(self.rows(hout, s, t), ho, q=k.pool)
                    self.pump()

                active = []
                for t in range(NTL + 2):
                    if t < NTL and t % 4 == 0:
                        scores(t // 4)
                    for g_ in list(active):
                        if next(g_, "done") == "done":
                            active.remove(g_)
                    if t < NTL:
                        g_ = tile_gen(t)
                        next(g_)
                        active.append(g_)
                for g_ in active:
                    for _ in g_:
                        pass

    def ffn(self, s, hin, hout):
        k = self.k
        TB = 1024
        NTB = TB // 128
        NJ = DFF // 128
        with k.scope() as es0:
            wd = k.sb([128, NJ, D], BF16, "wd", es0)
            self.wload(wd, "ff_w_down", 0, D)
            uT = k.sb([128, 8, TB], BF16, "uT", es0)
            hT = k.sb([128, NJ, TB], BF16, "hT", es0)
            wg = [k.sb([128, 8, 256], BF16, "wg", es0) for _ in range(2)]
            wu = [k.sb([128, 8, 256], BF16, "wu", es0) for _ in range(2)]
            sl = [k.sb([128, 512], F32, "sl", es0) for _ in range(2)]
            hts = [k.sb([128, D], F32, "hres", es0) for _ in range(2)]
            hos = [k.sb([128, D], F32, "hout", es0) for _ in range(2)]
            nbf = self.norm_bufs(es0, NTB, nh=2)
            for blk in range(S // TB):
                ub = [Buf() for _ in range(NTB)]
                hb = [[Buf() for _ in range(2)] for _ in range(NJ)]
                self.norm_T(nbf, hin, s, uT, ub, ntiles=NTB, tile0=blk * NTB)
                it = 0
                for jg in range(NJ // 2):
                    self.wload(wg[jg % 2], "ff_w_gu", jg * 256, 256)
                    self.wload(wu[jg % 2], "ff_w_gu", DFF + jg * 256, 256)
                    for jj in range(2):
                        j = jg * 2 + jj
                        for n in range(TB // 512):
                            pg, pu = k.psum(), k.psum()
                            for ps, w in ((pg, wg[jg % 2]), (pu, wu[jg % 2])):
                                for kc in range(8):
                                    k.mm(ps, w[:, kc, jj * 128:(jj + 1) * 128], uT[:, kc, n * 512:(n + 1) * 512].on(*ub[4 * n:4 * n + 4]),
                                         start=kc == 0, stop=kc == 7)
                            s_ = sl[it % 2]
                            it += 1
                            k.activation(s_, pg, AF.Silu)
                            k.tt(hT[:, j, n * 512:(n + 1) * 512].on(hb[j][n]), s_, pu, ALU.mult)
                            self.pump()
                for t in range(NTB):
                    gt = blk * NTB + t
                    ht, ho = hts[t % 2], hos[t % 2]
                    k.dma(ht, self.rows(hin, s, gt))
                    for nn in range(2):
                        ps = k.psum()
                        for j in range(NJ):
                            k.mm(ps, hT[:, j, t * 128:(t + 1) * 128].on(hb[j][t // 4]), wd[:, j, nn * 512:(nn + 1) * 512], start=j == 0, stop=j == NJ - 1)
                        k.tt(ho[:, nn * 512:(nn + 1) * 512], ps, ht[:, nn * 512:(nn + 1) * 512], ALU.add)
                    k.dma(self.rows(hout, s, gt), ho, q=k.pool)
                    self.pump()


    def deltanet(self, s, hin, hout):
        k = self.k
        NTL = S // 128
        cf = {n: self.c(n) for n in ("tri_incl", "blk64", "strict_ij", "causal_ji", "gtk", "ones", "ident")}
        import os
        DN = int(os.environ.get("DN_STOP", "99"))
        with k.scope() as es0:
            qkvT = k.sb([128, 24, S], BF16, "qkvT", es0)
            bg = k.sb([128, NTL, 16], F32, "bg", es0)
            nexpa = k.sb([128, 8], F32, "nexpa", es0)
            one1 = k.sb([128, 1], F32, "one1", es0)
            k.memset(one1, 1.0)
            k.activation(nexpa, self.c("od_alog"), AF.Exp)
            k.ts(nexpa, nexpa, -1.0, ALU.mult)
            with k.scope() as es1:
                uT = k.sb([128, 8, S], BF16, "uT", es1)
                ub = [Buf() for _ in range(NTL)]
                with k.scope() as es:
                    self.norm_T(es, hin, s, uT, ub)
                if DN <= -1:
                    return
                wba = k.sb([128, 8, 16], BF16, "wba", es1)
                self.wload(wba, "od_w_in", 4096, 16)
                sm = [k.sb([128, 40], F32, "bgt", es1) for _ in range(2)]
                for t in range(NTL):
                    ps = k.psum()
                    for kc in range(8):
                        k.mm(ps[:, 0:16], uT[:, kc, t * 128:(t + 1) * 128].on(ub[t]), wba[:, kc, :], start=kc == 0, stop=kc == 7)
                    w = sm[t % 2]
                    k.activation(bg[:, t, 0:8], ps[:, 0:8], AF.Sigmoid)
                    k.tt(w[:, 0:8], ps[:, 8:16], self.c("od_dtb"), ALU.add)
                    k.ts(w[:, 32:40], w[:, 0:8], -1.0, ALU.mult)
                    k.tt(w[:, 8:16], w[:, 0:8], w[:, 32:40], ALU.max)
                    k.activation(w[:, 16:24], w[:, 8:16], AF.Exp, scale=-1.0)
                    k.activation(w[:, 24:32], w[:, 16:24], AF.Ln, bias=one1)
                    k.ts(w[:, 0:8], w[:, 0:8], 0.0, ALU.max)
                    k.tt(w[:, 0:8], w[:, 0:8], w[:, 24:32], ALU.add)
                    k.tt(bg[:, t, 8:16], w[:, 0:8], nexpa, ALU.mult)
                if DN <= 0:
                    return
                wch = [k.sb([128, 8, 512], BF16, "wch", es1) for _ in range(2)]
                ys = [k.sb([128, S + 3], F32, "y", es1) for _ in range(2)]
                acc = [k.sb([128, S], F32, "acc", es1) for _ in range(2)]
                sq = [k.sb([128, 512], BF16, "sq", es1) for _ in range(2)]
                rr = [k.sb([128, 512], F32, "rr", es1) for _ in range(2)]
                for y in ys:
                    k.memset(y[:, 0:3], 0.0)
                cw = self.c("od_conv")
                it = 0
                for c in [int(x) for x in os.environ.get('DN_C', ','.join(map(str, range(24)))).split(',')]:
                    wc, y, a_ = wch[(c // 4) % 2][:, :, (c % 4) * 128:(c % 4 + 1) * 128], ys[c % 2], acc[c % 2]
                    if c % 4 == 0:
                        self.wload(wch[(c // 4) % 2], "od_w_in", c * 128, 512)
                    for n in range(4):
                        ps = k.psum()
                        for kc in range(8):
                            k.mm(ps, wc[:, kc, :], uT[:, kc, n * 512:(n + 1) * 512].on(*ub[4 * n:4 * n + 4]), start=kc == 0, stop=kc == 7)
                        k.copy(y[:, 3 + n * 512:3 + (n + 1) * 512], ps, e=k.act)
                    k.ts(a_, y[:, 3:S + 3], cw[:, c * 4 + 3:c * 4 + 4], ALU.mult)
                    for j in (2, 1, 0):
                        k.stt(a_, y[:, j:S + j], cw[:, c * 4 + j:c * 4 + j + 1], a_, ALU.mult, ALU.add)
                    if c >= 16:
                        k.activation(qkvT[:, c, :], a_, AF.Silu)
                    else:
                        k.activation(a_, a_, AF.Silu)
                        for n in range(4):
                            sl = slice(n * 512, (n + 1) * 512)
                            q_, r_ = sq[it % 2], rr[it % 2]
                            it += 1
                            k.activation(q_, a_[:, sl], AF.Square)
                            ps2 = k.psum()
                            k.mm(ps2, self.cb["ones"], q_)
                            k.activation(r_, ps2, AF.Ln, bias=self.eps)
                            k.activation(r_, r_, AF.Exp, scale=-0.5)
                            if c < 8:
                                k.stt(qkvT[:, c, sl], a_[:, sl], 128.0 ** -0.5, r_, ALU.mult, ALU.mult)
                            else:
                                k.tt(qkvT[:, c, sl], a_[:, sl], r_, ALU.mult)
            def emit_og(t, og):
                tk = slice(t * 128, (t + 1) * 128)
                k.dma(self.ogd[s][:, tk].rearrange("(h p) t -> p h t", p=128).on(Buf()), og, q=k.pool)
            self.dn_scan(qkvT, bg, NTL, emit_og)
        with k.scope() as es0:
            uT = k.sb([128, 8, S], BF16, "uT", es0)
            ub = [Buf() for _ in range(NTL)]
            self.norm_T(self.norm_bufs(es0, nh=2), hin, s, uT, ub)
            ogT = k.sb([128, 8, S], BF16, "ogT", es0)
            k.dma(ogT, self.ogd[s].rearrange("(h p) t -> p h t", p=128))
            wgt = [k.sb([128, 8, 512], BF16, "wgt", es0) for _ in range(2)]
            sg = [k.sb([128, 512], F32, "sg", es0) for _ in range(2)]
            it = 0
            for c in range(8):
                if c % 4 == 0:
                    self.wload(wgt[c // 4], "od_w_in", 3072 + c * 128, 512)
                for n in range(4):
                    ps = k.psum()
                    for kc in range(8):
                        k.mm(ps, wgt[c // 4][:, kc, (c % 4) * 128:(c % 4 + 1) * 128], uT[:, kc, n * 512:(n + 1) * 512].on(*ub[4 * n:4 * n + 4]), start=kc == 0, stop=kc == 7)
                    g_ = sg[it % 2]
                    it += 1
                    k.activation(g_, ps, AF.Silu)
                    k.tt(ogT[:, c, n * 512:(n + 1) * 512], ogT[:, c, n * 512:(n + 1) * 512], g_, ALU.mult)
            wout = k.sb([128, 8, D], BF16, "wout", es0)
            self.wload(wout, "od_w_out", 0, D)
            hts = [k.sb([128, D], F32, "hres", es0) for _ in range(2)]
            hos = [k.sb([128, D], F32, "hout", es0) for _ in range(2)]
            for t in range(NTL):
                ht, ho = hts[t % 2], hos[t % 2]
                k.dma(ht, self.rows(hin, s, t))
                for n in range(2):
                    ps = k.psum()
                    for c in range(8):
                        k.mm(ps, ogT[:, c, t * 128:(t + 1) * 128], wout[:, c, n * 512:(n + 1) * 512], start=c == 0, stop=c == 7)
                    k.tt(ho[:, n * 512:(n + 1) * 512], ps, ht[:, n * 512:(n + 1) * 512], ALU.add)
                k.dma(self.rows(hout, s, t), ho, q=k.pool)

    def dn_scan(self, qkvT, bg, ntiles, emit_og):
        import os
        SS = int(os.environ.get('SCAN_STOP', '99'))
        k = self.k
        tri_b, blk_b, gtk_b = self.dnc["tri_incl"], self.dnc["blk64"], self.dnc["gtk"]
        ones_b, ident_b = self.cb["ones"], self.cb["ident"]
        tri32, strict32, causal32 = self.c("tri_incl"), self.c("strict_ij"), self.c("causal_ji")
        v3 = lambda x: x.rearrange("p (h o) -> p h o", o=1)
        r3 = lambda x: x.rearrange("p (o c) -> p o c", o=1)
        with k.scope() as es1:
            S32 = k.sb([128, 8, 128], F32, "S32", es1)
            Sbf = k.sb([128, 8, 128], BF16, "Sbf", es1)
            Sb = [Buf() for _ in range(8)]
            Sbb = [Buf() for _ in range(8)]
            k.memset(S32, 0.0)
            k.memset(Sbf, 0.0)
            mk = lambda shp, dt, nm, n=2: [k.sb(shp, dt, nm, es1) for _ in range(n)]
            mk2 = lambda shp, dt, nm, n=2: [mk(shp, dt, nm, n) for _ in range(2)]
            tsm = mk([128, 64], F32, "tsm")
            gsp = mk([128, 16], BF16, "gsp")
            Tgh = mk2([128, 4, 128], BF16, "Tgh")
            Tgl = mk2([128, 4, 128], BF16, "Tgl")
            E1 = mk2([128, 4, 128], F32, "E1", 1)
            E2 = mk2([128, 4, 128], F32, "E2", 1)
            EG = mk2([128, 4, 128], F32, "EG")
            kb_ = mk2([128, 4, 128], BF16, "kb", 1)
            kd = mk2([128, 4, 128], BF16, "kd")
            vb = mk2([128, 4, 128], BF16, "vb", 1)
            qd = mk2([128, 4, 128], BF16, "qd")
            attT = mk2([128, 4, 128], BF16, "attT")
            X = mk2([128, 4, 384], BF16, "X", 1)
            Xb = [Buf() for _ in range(8)]
            uu = mk2([128, 4, 128], F32, "uu")
            wT = mk2([128, 4, 128], BF16, "wT")
            vn = mk([128, 128], BF16, "vn", 4)
            sqo = mk([128, 128], BF16, "sqo")
            ro = mk([128, 128], F32, "ro")
            ogs = mk([128, 8, 128], BF16, "ogs")

            def pre(t, hf):
                tk = slice(t * 128, (t + 1) * 128)
                p2_ = t % 2
                H0 = hf * 4
                w, gs = tsm[p2_], gsp[p2_]
                beta, g = bg[:, t, 0:8], bg[:, t, 8:16]
                if hf == 0:
                    k.copy(gs[:, 0:8], g)
                    k.copy(w[:, 32:40], gs[:, 0:8])
                    k.tt(w[:, 40:48], g, w[:, 32:40], ALU.subtract)
                    k.copy(gs[:, 8:16], w[:, 40:48])
                    ps = k.psum()
                    k.mm(ps[:, 0:8], tri_b, gs[:, 0:8], start=True, stop=False)
                    k.mm(ps[:, 0:8], tri_b, gs[:, 8:16], start=False, stop=True)
                    k.mm(ps[:, 8:16], blk_b, gs[:, 0:8], start=True, stop=False)
                    k.mm(ps[:, 8:16], blk_b, gs[:, 8:16], start=False, stop=True)
                    k.activation(w[:, 0:8], ps[:, 0:8], AF.Exp)
                    k.copy(w[:, 8:16], ps[:, 0:8])
                    k.tt(w[:, 16:24], ps[:, 8:16], w[:, 8:16], ALU.subtract)
                    k.activation(w[:, 16:24], w[:, 16:24], AF.Exp)
                    k.tt(w[:, 24:32], w[:, 0:8], beta, ALU.mult)
                yield
                wh = lambda c0: w[:, c0 + H0:c0 + H0 + 4]
                bh = beta[:, H0:H0 + 4]
                th, tl = Tgh[hf][p2_], Tgl[hf][p2_]
                k.tt(th, r3(tri32).bc([128, 4, 128]), v3(wh(32)).bc([128, 4, 128]), ALU.mult)
                k.tt(tl, r3(tri32).bc([128, 4, 128]), v3(wh(40)).bc([128, 4, 128]), ALU.mult, e=k.pool)
                yield
                e1, e2, eg = E1[hf][0], E2[hf][0], EG[hf][p2_]
                f2 = lambda v_: v_.rearrange("p h c -> p (h c)")
                pa = k.psum()
                k.mm(pa, gtk_b, f2(th), start=True, stop=False)
                k.mm(pa, gtk_b, f2(tl), start=False, stop=True)
                k.activation(f2(e2), pa, AF.Exp)
                pb = k.psum()
                k.mm(pb, ones_b, f2(th), start=True, stop=False)
                k.mm(pb, ones_b, f2(tl), start=False, stop=True)
                k.activation(f2(eg), pb, AF.Exp)
                yield
                pc = k.psum()
                for hh in range(4):
                    k.mm(pc[:, hh * 128:(hh + 1) * 128], th[:, hh, :], gtk_b, start=True, stop=False)
                    k.mm(pc[:, hh * 128:(hh + 1) * 128], tl[:, hh, :], gtk_b, start=False, stop=True)
                k.activation(f2(e1), pc, AF.Exp)
                yield
                k.tt(e2, e2, r3(causal32).bc([128, 4, 128]), ALU.mult, e=k.pool)
                k.tt(e1, e1, r3(strict32).bc([128, 4, 128]), ALU.mult)
                k.tt(e1, e1, v3(bh).bc([128, 4, 128]), ALU.mult)
                k.tt(qd[hf][p2_], qkvT[:, H0:H0 + 4, tk], eg, ALU.mult, e=k.pool)
                yield
                pkt = k.psum()
                pkb = pkt.bitcast(BF16)
                for hh in range(4):
                    k.tr(pkb[:, hh * 128:(hh + 1) * 128], qkvT[:, 8 + H0 + hh, tk], ident_b)
                for hh in range(4):
                    k.tr(pkb[:, 512 + hh * 128:512 + (hh + 1) * 128], qkvT[:, 16 + H0 + hh, tk], ident_b)
                pk3 = pkb[:, 0:512].rearrange("p (h c) -> p h c", h=4)
                pv3 = pkb[:, 512:1024].rearrange("p (h c) -> p h c", h=4)
                k.tt(kb_[hf][0], pk3, v3(wh(24)).bc([128, 4, 128]), ALU.mult)
                k.tt(kd[hf][p2_], pk3, v3(wh(16)).bc([128, 4, 128]), ALU.mult)
                k.tt(vb[hf][0], pv3, v3(bh).bc([128, 4, 128]), ALU.mult)
                yield
                x = X[hf][0]
                xb = Xb[H0:H0 + 4]
                for hp in range(2):
                    pk = k.psum()
                    for i in range(2):
                        h = H0 + hp * 2 + i
                        kT_, qT_ = qkvT[:, 8 + h, tk], qkvT[:, h, tk]
                        k.mm(pk[:, i * 256:i * 256 + 128], kT_, kT_)
                        k.mm(pk[:, i * 256 + 128:i * 256 + 256], kT_, qT_)
                    pk4 = pk.rearrange("p (h a c) -> p h a c", h=2, a=2)
                    hs = slice(hp * 2, hp * 2 + 2)
                    k.tt(x[:, hs, 256:384].on(xb[hp * 2], xb[hp * 2 + 1]), pk4[:, :, 0, :], e1[:, hs, :], ALU.mult)
                    k.tt(attT[hf][p2_][:, hs, :], pk4[:, :, 1, :], e2[:, hs, :], ALU.mult)
                    yield
                ptr = k.psum()
                ptb = ptr.bitcast(BF16)
                for hh in range(4):
                    k.tr(ptb[:, hh * 128:(hh + 1) * 128], x[:, hh, 256:384].on(xb[hh]), ident_b)
                pt3 = ptb[:, 0:512].rearrange("p (h c) -> p h c", h=4)
                k.copy(x[:, :, 128:256].on(*xb), pt3, e=k.act)
                k.tt(x[:, :, 0:128].on(*xb), r3(ident_b).bc([128, 4, 128]), pt3, ALU.subtract)
                yield
                for lvl in range(5):
                    for hh in range(4):
                        xh = x[:, hh, :].on(xb[hh])
                        ev = k.act if hh % 2 else k.dve
                        pl = k.psum()
                        if lvl == 0:
                            k.mm(pl[:, 128:256], xh[:, 256:384], xh[:, 128:256])
                            k.mm(pl[:, 256:384], xh[:, 128:256], xh[:, 256:384])
                            k.copy(xh[:, 128:384], pl[:, 128:384], e=ev)
                        else:
                            k.mm(pl[:, 0:256], xh[:, 256:384], xh[:, 0:256], start=True, stop=False)
                            k.mm(pl[:, 0:128], ident_b, xh[:, 0:128], start=False, stop=True)
                            k.mm(pl[:, 256:384], xh[:, 128:256], xh[:, 256:384])
                            k.copy(xh[:, 0:384], pl[:, 0:384], e=ev)
                        if hh % 2:
                            yield
                for hh in range(4):
                    xh = x[:, hh, :].on(xb[hh])
                    pl = k.psum()
                    k.mm(pl[:, 0:128], xh[:, 256:384], xh[:, 0:128], start=True, stop=False)
                    k.mm(pl[:, 0:128], ident_b, xh[:, 0:128], start=False, stop=True)
                    k.copy(xh[:, 0:128], pl[:, 0:128], e=k.dve if hh % 2 else k.act)
                    pu = k.psum()
                    k.mm(pu[:, 0:128], xh[:, 0:128], vb[hf][0][:, hh, :])
                    k.mm(pu[:, 128:256], kb_[hf][0][:, hh, :], xh[:, 0:128])
                    k.copy(uu[hf][p2_][:, hh, :], pu[:, 0:128], e=k.act)
                    k.copy(wT[hf][p2_][:, hh, :], pu[:, 128:256], e=k.act)
                    if hh % 2:
                        yield

            vit = [0]

            def scan(t):
                p2_ = t % 2
                po = [k.psum(hold=True), k.psum(hold=True)]
                for ch in range(2):
                    c0 = ch * 64
                    cs = slice(c0, c0 + 64)
                    for h in range(8):
                        hf, hh = h // 4, h % 4
                        eg = EG[hf][p2_]
                        vn_ = vn[vit[0] % 4]
                        vit[0] += 1
                        Sv = Sbf[:, h, :].on(Sbb[h])
                        p1 = k.psum()
                        k.mm(p1[cs, 0:128], wT[hf][p2_][:, hh, cs], Sv)
                        k.tt(vn_[cs, :], uu[hf][p2_][cs, hh, :], p1[cs, 0:128], ALU.subtract)
                        oreg = po[h // 4][:, (h % 4) * 128 + c0:(h % 4) * 128 + c0 + 64]
                        k.mm(oreg, Sv, qd[hf][p2_][:, hh, cs], start=True, stop=False)
                        yield
                        k.mm(oreg, vn_[cs, :], attT[hf][p2_][cs, hh, cs], start=False, stop=True)
                        p2 = k.psum()
                        k.mm(p2[:, 0:128], kd[hf][p2_][cs, hh, :], vn_[cs, :])
                        k.stt(S32[:, h, :].on(Sb[h]), S32[:, h, :].on(Sb[h]), eg[:, hh, c0 + 63:c0 + 64], p2[:, 0:128], ALU.mult, ALU.add)
                        k.copy(Sbf[:, h, :].on(Sbb[h]), S32[:, h, :].on(Sb[h]), e=k.act)
                        yield
                og = ogs[p2_]
                for h in range(8):
                    ov = po[h // 4][:, (h % 4) * 128:(h % 4 + 1) * 128]
                    q_, r_ = sqo[h % 2], ro[h % 2]
                    k.activation(q_, ov, AF.Square)
                    ps2 = k.psum()
                    k.mm(ps2[:, 0:128], ones_b, q_)
                    k.activation(r_, ps2[:, 0:128], AF.Ln, scale=1.0 / 128, bias=self.eps)
                    k.activation(r_, r_, AF.Exp, scale=-0.5)
                    k.stt(og[:, h, :], ov, self.c("od_on"), r_, ALU.mult, ALU.mult)
                    yield
                k.release(po[0])
                k.release(po[1])
                emit_og(t, og)

            def run_rr(gens):
                gens = list(gens)
                while gens:
                    for g_ in list(gens):
                        if next(g_, "done") == "done":
                            gens.remove(g_)

            g0, g1 = pre(0, 0), pre(0, 1)
            next(g0)
            run_rr([g0, g1])
            for t in range(ntiles):
                gens = [scan(t)]
                if t + 1 < ntiles:
                    g0, g1 = pre(t + 1, 0), pre(t + 1, 1)
                    next(g0)
                    gens += [g0, g1]
                run_rr(gens)

    def moe(self, s, hin, hout):
        k = self.k
        TB = 1024
        NTB = TB // 128
        NJ = DFE // 128
        with k.scope() as es0:
            uT = k.sb([128, 8, TB], BF16, "uT", es0)
            hT = k.sb([128, NJ, TB], BF16, "hT", es0)
            acc = k.sb([128, NTB, D], F32, "macc", es0)
            comb = k.sb([128, NTB, 8], F32, "comb", es0)
            wr = k.sb([128, 8, 8], BF16, "wr", es0)
            self.wload(wr, "moe_router", 0, 8)
            wd = [k.sb([128, NJ, D], BF16, "wd", es0) for _ in range(2)]
            wgh = [k.sb([128, 8, 768], BF16, "wgh", es0) for _ in range(2)]
            wuh = [k.sb([128, 8, 768], BF16, "wuh", es0) for _ in range(2)]
            sl = [k.sb([128, 512], BF16, "sl", es0) for _ in range(2)]
            rt = [k.sb([128, 48], F32, "rt", es0) for _ in range(2)]
            nbf = self.norm_bufs(es0, NTB, nh=2)
            for blk in range(S // TB):
                ub = [Buf() for _ in range(NTB)]
                ab = [Buf() for _ in range(NTB)]
                self.norm_T(nbf, hin, s, uT, ub, ntiles=NTB, tile0=blk * NTB)
                for t in range(NTB):
                    k.dma(acc[:, t, :].on(ab[t]), self.rows(hin, s, blk * NTB + t))
                    ps = k.psum()
                    for kc in range(8):
                        k.mm(ps[:, 0:8], uT[:, kc, t * 128:(t + 1) * 128].on(ub[t]), wr[:, kc, :], start=kc == 0, stop=kc == 7)
                    w = rt[t % 2]
                    k.copy(w[:, 0:8], ps[:, 0:8])
                    k.max8(w[:, 8:16], w[:, 0:8])
                    k.ts(w[:, 16:17], w[:, 8:9], -1.0, ALU.mult)
                    k.activation(w[:, 24:32], w[:, 0:8], AF.Exp, bias=w[:, 16:17])
                    k.ts(w[:, 32:40], w[:, 0:8], w[:, 9:10], ALU.is_ge)
                    k.tt(w[:, 24:32], w[:, 24:32], w[:, 32:40], ALU.mult)
                    k.reduce(w[:, 17:18], w[:, 24:32], ALU.add)
                    k.recip(w[:, 18:19], w[:, 17:18])
                    k.ts(comb[:, t, :], w[:, 24:32], w[:, 18:19], ALU.mult)
                it = 0
                HALF = ((0, 6), (6, NJ))

                def load_unit(u):
                    e_, hf = u // 2, u % 2
                    j0, j1 = HALF[hf]
                    self.wload(wgh[u % 2][:, :, 0:(j1 - j0) * 128], f"moe_w_gu{e_}", j0 * 128, (j1 - j0) * 128)
                    self.wload(wuh[u % 2][:, :, 0:(j1 - j0) * 128], f"moe_w_gu{e_}", DFE + j0 * 128, (j1 - j0) * 128)
                load_unit(0)
                self.wload(wd[0], "moe_w_down0", 0, D)
                for e in range(NEXP):
                    wde = wd[e % 2]
                    hb = [[Buf() for _ in range(2)] for _ in range(NJ)]
                    for hf in range(2):
                        u = e * 2 + hf
                        if u + 1 < 2 * NEXP:
                            load_unit(u + 1)
                        if hf == 0 and e + 1 < NEXP:
                            self.wload(wd[(e + 1) % 2], f"moe_w_down{e + 1}", 0, D)
                        j0, j1 = HALF[hf]
                        for j in range(j0, j1):
                            wg_ = wgh[u % 2][:, :, (j - j0) * 128:(j - j0 + 1) * 128]
                            wu_ = wuh[u % 2][:, :, (j - j0) * 128:(j - j0 + 1) * 128]
                            for n in range(TB // 512):
                                pg, pu = k.psum(), k.psum()
                                for ps, w_ in ((pg, wg_), (pu, wu_)):
                                    for kc in range(8):
                                        k.mm(ps, w_[:, kc, :], uT[:, kc, n * 512:(n + 1) * 512].on(*ub[4 * n:4 * n + 4]),
                                             start=kc == 0, stop=kc == 7)
                                s_ = sl[it % 2]
                                it += 1
                                k.activation(s_, pg, AF.Silu)
                                k.tt(hT[:, j, n * 512:(n + 1) * 512].on(hb[j][n]), s_, pu, ALU.mult)
                    for t in range(NTB):
                        for nn in range(2):
                            ps = k.psum()
                            for j in range(NJ):
                                k.mm(ps, hT[:, j, t * 128:(t + 1) * 128].on(hb[j][t // 4]), wde[:, j, nn * 512:(nn + 1) * 512], start=j == 0, stop=j == NJ - 1)
                            a_ = acc[:, t, nn * 512:(nn + 1) * 512].on(ab[t])
                            k.stt(a_, ps, comb[:, t, e:e + 1], a_, ALU.mult, ALU.add)
                for t in range(NTB):
                    k.dma(self.rows(hout, s, blk * NTB + t), acc[:, t, :].on(ab[t]), q=k.pool)


_CACHE = {}


def _run(inputs, nseq, ncores, stop="end"):
    key = (nseq, stop)
    if key not in _CACHE:
        p = Prog(nseq, stop)
        _CACHE[key] = (p.build(), p)
    nc = _CACHE[key][0]
    cst = make_consts(inputs)
    hw = host_weights(inputs)
    x = np.asarray(inputs["x"], dtype=np.float32)
    mem = np.asarray(inputs["mem"], dtype=np.float32)
    in_maps = []
    for c in range(ncores):
        m = {"x": np.ascontiguousarray(x[c * nseq:(c + 1) * nseq].reshape(nseq * S, D)),
             "mem": np.ascontiguousarray(mem[c * nseq:(c + 1) * nseq].reshape(nseq * NMEM, D)),
             "cst": cst}
        m.update(hw)
        in_maps.append(m)
    res = run_bass_kernel_spmd(nc, in_maps, core_ids=list(range(ncores)))
    global _LAST
    _LAST = res
    return np.concatenate([r["out"].reshape(nseq, S, D) for r in res.results], axis=0)


def kernel(**inputs):
    return _run(inputs, 4, 8)
```
